# Optimizing a Trainium2 kernel written in Bass

```python
import math
import jax, jax.numpy as jnp
from jax import lax
import numpy as np

D_MODEL = 2048
BATCH = 4
SEQ = 2048
DEPTH = 2

A_HEADS = 16
A_KV_HEADS = 4
A_HEAD_DIM = 128
IDX_HEADS = 16
IDX_DIM = 64
TOPK_MAX = 256
B_DILATIONS = ((128, 1), (512, 4), (2048, 16))
N_GROUPS = 3
B_HEADS = 16
B_HEAD_DIM = 64
N_BUCKETS = 32
MAX_DISTANCE = 2048
BIAS_HEADS = 16
D_FF = 5632
N_EXPERTS = 8
TOP_K_EXPERTS = 2
D_FF_EXPERT = 7168
QBLOCK = 128
EPS = 1e-6

N_A = DEPTH // 2
N_B = DEPTH - N_A
N_DENSE = (DEPTH + 1) // 2
N_MOE = DEPTH // 2
A_Q = A_HEADS * A_HEAD_DIM
A_KV = A_KV_HEADS * A_HEAD_DIM
A_QI = IDX_HEADS * IDX_DIM
A_IN = A_Q + 2 * A_KV + A_QI + IDX_DIM + IDX_HEADS
B_Q = N_GROUPS * B_HEADS * B_HEAD_DIM
B_OUT = B_HEADS * B_HEAD_DIM

kernel_name = "yoco_dsa_dilated_moe_trunk"


def rmsnorm(x, g):
    xf = x.astype(jnp.float32)
    y = xf * lax.rsqrt(jnp.mean(xf * xf, axis=-1, keepdims=True) + EPS)
    return (y * g.astype(jnp.float32)).astype(x.dtype)


def modulate(h, shift, scale):
    return h * (1 + scale[:, None, :]) + shift[:, None, :]


def t5_bucket(rel):
    n = jnp.maximum(rel, 0)
    max_exact = N_BUCKETS // 2
    nf = jnp.maximum(n, 1).astype(jnp.float32)
    large = max_exact + (jnp.log(nf / max_exact) / math.log(MAX_DISTANCE / max_exact)
                         * (N_BUCKETS - max_exact)).astype(jnp.int32)
    large = jnp.minimum(large, N_BUCKETS - 1)
    return jnp.where(n < max_exact, n, large)


def swiglu(h, w1, w3, w2):
    return (jax.nn.silu(h @ w1) * (h @ w3)) @ w2


def dsa_attention(h, pos, w_in, w_out, g_qn, g_kn, rel_bias):
    b_, s_len, _ = h.shape
    proj = h @ w_in
    cuts = [A_Q, A_Q + A_KV, A_Q + 2 * A_KV, A_Q + 2 * A_KV + A_QI, A_Q + 2 * A_KV + A_QI + IDX_DIM]
    q, k, v, qi, ki, wi = jnp.split(proj, cuts, axis=-1)
    q = rmsnorm(q.reshape(b_, s_len, A_HEADS, A_HEAD_DIM), g_qn)
    k = rmsnorm(k.reshape(b_, s_len, A_KV_HEADS, A_HEAD_DIM), g_kn)
    v = v.reshape(b_, s_len, A_KV_HEADS, A_HEAD_DIM)
    qi = qi.reshape(b_, s_len, IDX_HEADS, IDX_DIM)
    wi = wi * (IDX_HEADS ** -0.5)
    topk = min(TOPK_MAX, s_len // 4)
    nq = s_len // QBLOCK
    grp = A_HEADS // A_KV_HEADS
    scale = A_HEAD_DIM ** -0.5

    def to_blocks(a):
        return jnp.moveaxis(a.reshape((b_, nq, QBLOCK) + a.shape[2:]), 1, 0)

    def block(args):
        bi, qb, qib, wib, qpos = args
        t = bi * QBLOCK + jnp.arange(QBLOCK)
        causal = jnp.arange(s_len)[None, :] <= t[:, None]
        isc = jnp.einsum('bqhd,bsd->bqhs', qib, ki, preferred_element_type=jnp.float32)
        isc = jnp.einsum('bqhs,bqh->bqs', jax.nn.relu(isc) * (IDX_DIM ** -0.5),
                         wib.astype(jnp.float32))
        isc = jnp.where(causal[None], isc, -jnp.inf)
        _, sel = lax.top_k(isc, topk)
        valid = sel <= t[None, :, None]
        flat = sel.reshape(b_, QBLOCK * topk)
        kg = jnp.take_along_axis(k, flat[:, :, None, None], axis=1).reshape(
            b_, QBLOCK, topk, A_KV_HEADS, A_HEAD_DIM)
        vg = jnp.take_along_axis(v, flat[:, :, None, None], axis=1).reshape(
            b_, QBLOCK, topk, A_KV_HEADS, A_HEAD_DIM)
        kpos = jnp.take_along_axis(pos, flat, axis=1).reshape(b_, QBLOCK, topk)
        bias = rel_bias[t5_bucket(qpos[:, :, None] - kpos)]
        bias = jnp.transpose(bias.reshape(b_, QBLOCK, topk, A_KV_HEADS, grp), (0, 1, 3, 4, 2))
        qg = qb.reshape(b_, QBLOCK, A_KV_HEADS, grp, A_HEAD_DIM)
        logits = jnp.einsum('bqkgd,bqjkd->bqkgj', qg, kg, preferred_element_type=jnp.float32) * scale + bias
        logits = jnp.where(valid[:, :, None, None, :], logits, -jnp.inf)
        p = jax.nn.softmax(logits, axis=-1)
        o = jnp.einsum('bqkgj,bqjkd->bqkgd', p.astype(vg.dtype), vg)
        return o.reshape(b_, QBLOCK, A_Q)

    outs = lax.map(block, (jnp.arange(nq), to_blocks(q), to_blocks(qi), to_blocks(wi), to_blocks(pos)))
    o = jnp.moveaxis(outs, 0, 1).reshape(b_, s_len, A_Q)
    return o @ w_out


def dilated_group(q, k, v, pos, window, r, rel_bias):
    b_, s_len, nh, hd = q.shape
    wk = window // r
    n = s_len // r
    bq = math.gcd(QBLOCK, n)
    nb = n // bq
    qs = q.reshape(b_, nb, bq, r, nh, hd)
    pad5 = ((0, 0), (wk, 0), (0, 0), (0, 0), (0, 0))
    kp = jnp.pad(k.reshape(b_, n, r, nh, hd), pad5)
    vp = jnp.pad(v.reshape(b_, n, r, nh, hd), pad5)
    pp = jnp.pad(pos.reshape(b_, n, r), ((0, 0), (wk, 0), (0, 0)))
    idx = np.arange(nb)[:, None] * bq + np.arange(bq + wk)[None, :]
    kb = kp[:, idx]
    vb = vp[:, idx]
    kpos = pp[:, idx]
    qpos = pos.reshape(b_, nb, bq, r)
    i = np.arange(bq)[:, None]
    j = np.arange(bq + wk)[None, :]
    band = (j >= i) & (j <= i + wk)
    start_ok = (np.arange(nb)[:, None, None] * bq + j[None] - wk) >= 0
    mask = jnp.asarray(band[None] & start_ok)
    logits = jnp.einsum('bnqrhd,bnkrhd->bnrhqk', qs, kb,
                        preferred_element_type=jnp.float32) * (hd ** -0.5)
    rel = qpos[:, :, :, None, :] - kpos[:, :, None, :, :]
    bias = rel_bias[t5_bucket(rel)]
    logits = logits + jnp.transpose(bias, (0, 1, 4, 5, 2, 3))
    logits = jnp.where(mask[None, :, None, None], logits, -jnp.inf)
    m = jnp.max(logits, axis=-1, keepdims=True)
    p = jnp.exp(logits - m)
    den = jnp.sum(p, axis=-1)
    num = jnp.einsum('bnrhqk,bnkrhd->bnrhqd', p.astype(vb.dtype), vb,
                     preferred_element_type=jnp.float32)
    num = jnp.transpose(num, (0, 1, 4, 2, 3, 5)).reshape(b_, s_len, nh, hd)
    den = jnp.transpose(den, (0, 1, 4, 2, 3)).reshape(b_, s_len, nh)
    m = jnp.transpose(m[..., 0], (0, 1, 4, 2, 3)).reshape(b_, s_len, nh)
    return num, den, m


def dilated_attention(h, pos, k_sh, v_sh, w_q, w_out, g_qn, rel_bias):
    b_, s_len, _ = h.shape
    q = rmsnorm((h @ w_q).reshape(b_, s_len, N_GROUPS, B_HEADS, B_HEAD_DIM), g_qn)
    nums, dens, ms = [], [], []
    for g, (window, r) in enumerate(B_DILATIONS):
        num, den, m = dilated_group(q[:, :, g], k_sh[:, :, g], v_sh[:, :, g], pos, window, r, rel_bias)
        nums.append(num); dens.append(den); ms.append(m)
    m_all = jnp.maximum(jnp.maximum(ms[0], ms[1]), ms[2])
    wts = [jnp.exp(mg - m_all) for mg in ms]
    num_tot = nums[0] * wts[0][..., None] + nums[1] * wts[1][..., None] + nums[2] * wts[2][..., None]
    den_tot = dens[0] * wts[0] + dens[1] * wts[1] + dens[2] * wts[2]
    o = (num_tot / den_tot[..., None]).astype(h.dtype).reshape(b_, s_len, B_OUT)
    return o @ w_out


def moe_swiglu(h, router, router_b, w1, w3, w2):
    logits = (h @ router).astype(jnp.float32) + router_b.astype(jnp.float32)
    topv, topi = lax.top_k(logits, TOP_K_EXPERTS)
    gates = jax.nn.softmax(topv, axis=-1)
    dense_gates = jnp.sum(jax.nn.one_hot(topi, N_EXPERTS, dtype=jnp.float32) * gates[..., None], axis=-2)
    out = jnp.zeros_like(h)
    for e in range(N_EXPERTS):
        out = out + dense_gates[..., e:e + 1].astype(h.dtype) * swiglu(h, w1[e], w3[e], w2[e])
    return out


def setup_inputs(seed: int = 0) -> dict:
    key = jax.random.key(seed)
    ks = jax.random.split(key, 32)

    def nrm(k, shape, scale):
        return jax.random.normal(k, shape, jnp.float32) * scale

    def gain(k, shape):
        return 1.0 + 0.02 * jax.random.normal(k, shape, jnp.float32)

    d = D_MODEL
    pos = (jnp.arange(SEQ, dtype=jnp.int32)[None, :]
           + jax.random.randint(ks[2], (BATCH, 1), 0, 4096, dtype=jnp.int32))
    return {
        "x": nrm(ks[0], (BATCH, SEQ, d), 1.0),
        "c": nrm(ks[1], (BATCH, d), 1.0),
        "positions": pos,
        "rel_bias": nrm(ks[3], (N_BUCKETS, BIAS_HEADS), 0.5),
        "w_mod": nrm(ks[4], (DEPTH, d, 6 * d), 0.5 * d ** -0.5),
        "b_mod": nrm(ks[5], (DEPTH, 6 * d), 0.01),
        "g_attn": gain(ks[6], (DEPTH, d)),
        "g_ffn": gain(ks[7], (DEPTH, d)),
        "a_w_in": nrm(ks[8], (N_A, d, A_IN), d ** -0.5),
        "a_w_out": nrm(ks[9], (N_A, A_Q, d), A_Q ** -0.5),
        "a_g_qn": gain(ks[10], (N_A, A_HEAD_DIM)),
        "a_g_kn": gain(ks[11], (N_A, A_HEAD_DIM)),
        "kv_w_mod": nrm(ks[12], (d, 2 * d), 0.5 * d ** -0.5),
        "kv_b_mod": nrm(ks[13], (2 * d,), 0.01),
        "kv_g": gain(ks[14], (d,)),
        "kv_w": nrm(ks[15], (d, 2 * B_Q), d ** -0.5),
        "b_g_kn": gain(ks[16], (B_HEAD_DIM,)),
        "b_w_q": nrm(ks[17], (N_B, d, B_Q), d ** -0.5),
        "b_w_out": nrm(ks[18], (N_B, B_OUT, d), B_OUT ** -0.5),
        "b_g_qn": gain(ks[19], (N_B, B_HEAD_DIM)),
        "ffn_w1": nrm(ks[20], (N_DENSE, d, D_FF), d ** -0.5),
        "ffn_w3": nrm(ks[21], (N_DENSE, d, D_FF), d ** -0.5),
        "ffn_w2": nrm(ks[22], (N_DENSE, D_FF, d), D_FF ** -0.5),
        "moe_router": nrm(ks[23], (N_MOE, d, N_EXPERTS), d ** -0.5),
        "moe_router_b": nrm(ks[24], (N_MOE, N_EXPERTS), 0.01),
        "moe_w1": nrm(ks[25], (N_MOE, N_EXPERTS, d, D_FF_EXPERT), d ** -0.5),
        "moe_w3": nrm(ks[26], (N_MOE, N_EXPERTS, d, D_FF_EXPERT), d ** -0.5),
        "moe_w2": nrm(ks[27], (N_MOE, N_EXPERTS, D_FF_EXPERT, d), D_FF_EXPERT ** -0.5),
    }


def reference(x, c, positions, rel_bias, w_mod, b_mod, g_attn, g_ffn, a_w_in, a_w_out, a_g_qn,
              a_g_kn, kv_w_mod, kv_b_mod, kv_g, kv_w, b_g_kn, b_w_q, b_w_out, b_g_qn, ffn_w1,
              ffn_w3, ffn_w2, moe_router, moe_router_b, moe_w1, moe_w3, moe_w2):
    b_, s_len, _ = x.shape
    cs = jax.nn.silu(c)
    k_sh = v_sh = None
    for l in range(DEPTH):
        mod = cs @ w_mod[l] + b_mod[l]
        sh1, sc1, gt1, sh2, sc2, gt2 = jnp.split(mod, 6, axis=-1)
        if l < N_A:
            h = modulate(rmsnorm(x, g_attn[l]), sh1, sc1)
            a = dsa_attention(h, positions, a_w_in[l], a_w_out[l], a_g_qn[l], a_g_kn[l], rel_bias)
        else:
            if l == N_A:
                kv_sh, kv_sc = jnp.split(cs @ kv_w_mod + kv_b_mod, 2, axis=-1)
                hkv = modulate(rmsnorm(x, kv_g), kv_sh, kv_sc)
                kv = (hkv @ kv_w).reshape(b_, s_len, 2, N_GROUPS, B_HEADS, B_HEAD_DIM)
                k_sh = rmsnorm(kv[:, :, 0], b_g_kn)
                v_sh = kv[:, :, 1]
            bi = l - N_A
            h = modulate(rmsnorm(x, g_attn[l]), sh1, sc1)
            a = dilated_attention(h, positions, k_sh, v_sh, b_w_q[bi], b_w_out[bi], b_g_qn[bi], rel_bias)
        x = x + gt1[:, None, :] * a
        h = modulate(rmsnorm(x, g_ffn[l]), sh2, sc2)
        if l % 2 == 0:
            f = swiglu(h, ffn_w1[l // 2], ffn_w3[l // 2], ffn_w2[l // 2])
        else:
            f = moe_swiglu(h, moe_router[l // 2], moe_router_b[l // 2], moe_w1[l // 2],
                           moe_w3[l // 2], moe_w2[l // 2])
        x = x + gt2[:, None, :] * f
    return x
```

```python
import math
from contextlib import ExitStack

import numpy as np
import ml_dtypes
import concourse.bass as bass
import concourse.mybir as mybir
from concourse.bass_utils import run_bass_kernel_spmd

F32 = mybir.dt.float32
BF16 = mybir.dt.bfloat16
I32 = mybir.dt.int32
AF = mybir.ActivationFunctionType
ALU = mybir.AluOpType
AX = mybir.AxisListType

D = 2048
SEQ = 2048
NB = 16
OWN = 8
TOWN = 1024
EPS = 1e-6
NEG = -30000.0
BIGNEG = -1.0e30


class Tk:
    __slots__ = ("name", "w", "rs")

    def __init__(self, name):
        self.name = name
        self.w = None
        self.rs = []


class Buf:
    def __init__(self, t, name, n=1):
        self.t = t
        self.name = name
        self.tk = Tk(name)
        self.sub = {}

    def __getitem__(self, k):
        return self.t[k]

    def k(self, key):
        if key not in self.sub:
            self.sub[key] = Tk(f"{self.name}.{key}")
        return self.sub[key]


def _tk(x):
    return x.tk if isinstance(x, Buf) else x


class Op:
    __slots__ = ("eng", "fn", "deps", "dma", "sig", "tok", "cnt")


NDSEM = 12


class Sched:
    ENGS = ("pe", "act", "dve", "pool", "sp")

    def __init__(self, nc):
        self.nc = nc
        self.es = ExitStack()
        self.sem = {e: self.es.enter_context(nc.semaphore(f"s_{e}")) for e in self.ENGS}
        self.dsem = {q: [self.es.enter_context(nc.semaphore(f"d_{q}{i}")) for i in range(NDSEM)]
                     for q in ("sp", "act", "pool")}
        self.cnt = {e: 0 for e in self.ENGS}
        self.ndma = {q: 0 for q in ("sp", "act", "pool")}
        self.ops = {e: [] for e in self.ENGS}
        self.trackers = []
        self.pending = {e: set() for e in self.ENGS}
        self.seen = {e: {} for e in self.ENGS}
        self.phase_es = None
        self.nops = 0
        self.prefix = ""
        self.ext_cache = {}

    def begin(self):
        self.phase_es = ExitStack()

    def sb(self, name, shape, dtype):
        name = self.prefix + name
        t = self.phase_es.enter_context(self.nc.sbuf_tensor(name, list(shape), dtype))
        return Buf(t, name)

    def ps(self, name, shape, dtype=F32):
        name = self.prefix + name
        t = self.phase_es.enter_context(self.nc.psum_tensor(name, list(shape), dtype))
        return Buf(t, name)

    def dram(self, name, shape, dtype, kind=None):
        if kind == "ExternalInput":
            if name in self.ext_cache:
                return self.ext_cache[name]
        if kind is None:
            name = self.prefix + name
            t = self.nc.dram_tensor(name, list(shape), dtype)
        else:
            t = self.nc.dram_tensor(name, list(shape), dtype, kind=kind)
        b = Buf(t.ap(), name)
        if kind == "ExternalInput":
            self.ext_cache[name] = b
        return b

    def op(self, eng, fn, r=(), w=(), dma=False, sig=True):
        o = Op()
        o.eng = eng
        o.fn = fn
        o.dma = dma
        o.sig = sig
        deps = set(self.pending[eng])
        self.pending[eng] = set()
        rt = [_tk(x) for x in r]
        wt = [_tk(x) for x in w]
        for t in rt:
            if t.w is not None:
                deps.add(t.w)
        for t in wt:
            if t.w is not None:
                deps.add(t.w)
            for x in t.rs:
                deps.add(x)
        idx = len(self.ops[eng])
        if dma:
            n = self.ndma[eng]
            self.ndma[eng] += 1
            tok = ("d", eng, n)
            if n >= NDSEM:
                deps.add(("d", eng, n - NDSEM))
        else:
            tok = ("e", eng, idx)
        o.tok = tok
        best = {}
        out = []
        for d in deps:
            if d[0] == "e":
                if d[1] == "pe" and eng == "pe" and not dma:
                    continue
                if d[1] not in best or best[d[1]] < d[2]:
                    best[d[1]] = d[2]
            else:
                out.append(d)
        for e_, i_ in best.items():
            out.append(("e", e_, i_))
            self.ops[e_][i_].sig = True
        o.deps = out
        for t in rt:
            t.rs = [x for x in t.rs if not (x[0] == "e" and tok[0] == "e" and x[1] == tok[1])] + [tok]
            if t not in self.trackers:
                self.trackers.append(t)
        for t in wt:
            t.w = tok
            t.rs = []
            if t not in self.trackers:
                self.trackers.append(t)
        self.ops[eng].append(o)
        self.nops += 1
        return o

    def mm(self, out, lhsT, rhs, start=True, stop=True, r=(), w=(), sig=None):
        if sig is None:
            sig = stop
        return self.op("pe", lambda e: e.matmul(out, lhsT, rhs, start=start, stop=stop), r, w, sig=sig)

    def tr(self, out, in_, ident, r=(), w=()):
        return self.op("pe", lambda e: e.transpose(out, in_, ident), r, w)

    def act(self, out, in_, func, r=(), w=(), **kw):
        return self.op("act", lambda e: e.activation(out, in_, func, **kw), r, w)

    def v(self, eng, name, *a, r=(), w=(), **kw):
        return self.op(eng, lambda e: getattr(e, name)(*a, **kw), r, w)

    def dma(self, q, out, in_, r=(), w=(), **kw):
        return self.op(q, lambda e: e.dma_start(out=out, in_=in_, **kw), r, w, dma=True)

    def nops_pending(self):
        return sum(len(v) for v in self.ops.values())

    def _engine(self, block, e):
        return {"pe": block.tensor, "act": block.scalar, "dve": block.vector,
                "pool": block.gpsimd, "sp": block.sync}[e]

    def end(self, final=False):
        self.flush()
        self.phase_es.close()
        self.phase_es = None

    def flush(self, final=False):
        if self.nops_pending() == 0:
            return
        pe_ops = self.ops["pe"]
        if pe_ops:
            pe_ops[-1].sig = True
        for e in self.ENGS:
            if self.ops[e] and not self.ops[e][-1].dma:
                self.ops[e][-1].sig = True
        nxt = [None] * len(pe_ops)
        last = None
        for i in range(len(pe_ops) - 1, -1, -1):
            if pe_ops[i].sig:
                last = i
            nxt[i] = last
        cntmap = {}
        for e in self.ENGS:
            c = self.cnt[e]
            for i, o in enumerate(self.ops[e]):
                if o.dma:
                    continue
                if o.sig:
                    c += 1
                    o.cnt = c
                    cntmap[(e, i)] = c
            self.cnt[e] = c

        def resolve(tok):
            if tok[0] == "d":
                _, q, n = tok
                return self.dsem[q][n % NDSEM], 16 * (n // NDSEM + 1)
            _, e, i = tok
            if e == "pe" and (e, i) not in cntmap:
                i = nxt[i]
            return self.sem[e], cntmap[(e, i)]

        base_idx = dict(self.phase_base) if hasattr(self, "phase_base") else {}

        def emit(e, eng):
            seen = self.seen[e]
            for o in self.ops[e]:
                for d in o.deps:
                    if d[0] == "e" and d[2] < 0:
                        continue
                    sem, val = resolve(d)
                    key = sem.num if hasattr(sem, "num") else id(sem)
                    if seen.get(key, 0) >= val:
                        continue
                    seen[key] = val
                    eng.wait_ge(sem, val)
                ins = o.fn(eng)
                if o.dma:
                    _, q, n = o.tok
                    ins.then_inc(self.dsem[q][n % NDSEM], 16)
                elif o.sig:
                    ins.then_inc(self.sem[e], 1)

        with self.nc.Block() as block:
            for e in self.ENGS:
                if not self.ops[e]:
                    continue

                def f(eng, e=e):
                    emit(e, eng)
                self._engine(block, e)(f)

        allt = set()
        for e in self.ENGS:
            if self.cnt[e] > 0:
                allt.add(("c", e, self.cnt[e]))
        for q in ("sp", "act", "pool"):
            n = self.ndma[q]
            for k in range(max(0, n - NDSEM), n):
                allt.add(("d", q, k))
        self.barrier_tokens = allt
        for e in self.ENGS:
            self.ops[e] = []
        for t in self.trackers:
            t.w = None
            t.rs = []
        self.trackers = []
        self._emit_barrier(final)

    def _emit_barrier(self, final):
        with self.nc.Block() as block:
            for e in self.ENGS:
                def f(eng, e=e):
                    seen = self.seen[e]
                    for t in self.barrier_tokens:
                        if t[0] == "c":
                            sem, val = self.sem[t[1]], t[2]
                        else:
                            _, q, n = t
                            sem, val = self.dsem[q][n % NDSEM], 16 * (n // NDSEM + 1)
                        key = sem.num if hasattr(sem, "num") else id(sem)
                        if seen.get(key, 0) >= val:
                            continue
                        seen[key] = val
                        eng.wait_ge(sem, val)
                self._engine(block, e)(f)


class Scope:
    def __init__(self, S):
        self.S = S
        self.stack = []

    def push(self):
        es = ExitStack()
        self.stack.append(es)
        self.S.phase_es = es

    def pop(self):
        self.S.flush()
        self.stack.pop().close()
        self.S.phase_es = self.stack[-1] if self.stack else None


def t5_thresholds():
    n = np.arange(0, 8192, dtype=np.int64)
    nf = np.maximum(n, 1).astype(np.float32)
    large = 16 + (np.log(nf / np.float32(16)) / np.float32(math.log(2048 / 16))
                  * np.float32(16)).astype(np.int32)
    large = np.minimum(large, 31)
    b = np.where(n < 16, n, large)
    T = [int(np.argmax(b >= k)) for k in range(32)]
    return T


NCONST = 400


def make_consts(parity):
    c = np.zeros((128, NCONST), np.float32)
    c[:, 0:128] = np.eye(128, dtype=np.float32)
    c[0:64, 128:192] = 1.0
    c[64:128, 192:256] = 1.0
    p = np.arange(128)
    for j in range(8):
        c[:, 256 + j] = (2 * j + parity) * 128 + p
    c[:, 264] = 128.0 * (2 * parity - 1)
    T = t5_thresholds()
    for b in range(32):
        c[b, 265] = T[b]
        c[b, 266] = T[b + 1] if b < 31 else 1e9
    c[:, 267] = p
    c[:, 268] = 1.0
    c[:, 269] = EPS
    c[:, 270] = float(parity)
    c[:, 271] = float(1 - parity)
    c[:, 272:400] = np.eye(128, dtype=np.float32)[::-1]
    return c


def make_relrow():
    return np.ascontiguousarray(np.broadcast_to((np.arange(2304, dtype=np.float32) - 256.0)[None, :], (32, 2304)))


def make_kidx(parity):
    k = np.zeros((1, 2048), np.float32)
    for m in range(8):
        k[0, m * 128:(m + 1) * 128] = (2 * m + parity) * 128 + np.arange(128)
        k[0, 1024 + m * 128:1024 + (m + 1) * 128] = (2 * m + 1 - parity) * 128 + np.arange(128)
    return k


def storage_order(parity):
    return [2 * m + parity for m in range(8)] + [2 * m + 1 - parity for m in range(8)]


WQ = ("sp", "act")


def mod_rows(S, SC, cs, W, Bv, N, out_fm, ident1, name):
    SC.push()
    wt = [S.sb(f"{name}_wt{i}", [128, 1024], F32) for i in range(4)]
    row = S.sb(f"{name}_row", [1, N], F32)
    brow = S.sb(f"{name}_brow", [1, N], F32)
    prow = [S.ps(f"{name}_pr{i}", [1, 1024], F32) for i in range(2)]
    pf = S.ps(f"{name}_pf", [128, 128], F32)
    S.dma("sp", brow[:, :], Bv[:, :], r=[Bv], w=[brow])
    n = 0
    for nb in range(N // 1024):
        pr = prow[nb % 2]
        for k in range(16):
            t = wt[n % 4]
            S.dma(WQ[n % 2], t[:, :], W[k * 128:(k + 1) * 128, nb * 1024:(nb + 1) * 1024], r=[W], w=[t])
            n += 1
            for h in range(2):
                S.mm(pr[0:1, h * 512:(h + 1) * 512], cs[:, k:k + 1], t[:, h * 512:(h + 1) * 512],
                     start=(k == 0), stop=(k == 15), r=[cs, t], w=[pr], sig=(k == 15 and h == 1) or True)
        S.v("dve", "tensor_tensor", row[0:1, nb * 1024:(nb + 1) * 1024], pr[0:1, :],
            brow[0:1, nb * 1024:(nb + 1) * 1024], ALU.add, r=[pr, brow], w=[row])
    nj = N // 128
    for j in range(nj):
        S.mm(pf[:, j:j + 1], row[0:1, j * 128:(j + 1) * 128], ident1[0:1, 0:1], start=True, stop=True,
             r=[row, ident1], w=[pf])
    S.v("dve", "tensor_copy", out_fm[:, 0:nj], pf[:, 0:nj], r=[pf], w=[out_fm])
    SC.pop()


def rms_fm(S, xT, ncols, onesb, out_rstd, sq, pss, epsc):
    for half in range(ncols // 512):
        cs_ = slice(half * 512, (half + 1) * 512)
        ps_ = pss[half % len(pss)]
        for c in range(16):
            q = sq[c % len(sq)]
            S.act(q[:, :], xT[:, c, cs_], AF.Square, r=[xT], w=[q])
            S.mm(ps_[:, :], onesb[:, :], q[:, :], start=(c == 0), stop=(c == 15), r=[onesb, q], w=[ps_])
        S.act(out_rstd[:, cs_], ps_[:, :], AF.Sqrt, scale=1.0 / 2048, bias=epsc, r=[ps_], w=[out_rstd])
        S.v("dve", "reciprocal", out_rstd[:, cs_], out_rstd[:, cs_], r=[out_rstd], w=[out_rstd])


def norm_mod_fm(S, xT, ncols, rstd, scol, bcol, outT, tmp, name):
    n = 0
    for half in range(ncols // 512):
        cs_ = slice(half * 512, (half + 1) * 512)
        for c in range(16):
            t = tmp[n % len(tmp)]
            n += 1
            S.v("dve", "scalar_tensor_tensor", t[:, :], xT[:, c, cs_], scol[:, c:c + 1], rstd[:, cs_],
                ALU.mult, ALU.mult, r=[xT, scol, rstd], w=[t])
            S.act(outT[:, c, cs_], t[:, :], AF.Identity, bias=bcol[:, c:c + 1], scale=1.0,
                  r=[t, bcol], w=[outT])


def wload(S, q, dst, dst_ap, W, c0, n, r0=0, nk=16):
    src = W[r0:r0 + nk * 128, c0:c0 + n].rearrange("(k p) n -> p k n", p=128)
    S.dma(q, dst_ap, src, r=[W], w=[dst])


def build_A(stop_after=None, debug=(), ctx=None):
    if ctx is None:
        nc = bass.Bass("TRN2", target_bir_lowering=False)
        S = Sched(nc)
        SC = Scope(S)
    else:
        nc, S, SC = ctx["nc"], ctx["S"], ctx["SC"]
    S.prefix = "za_"
    dbg = {}

    def ext(name, shape, dt=F32):
        return S.dram(name, shape, dt, kind="ExternalInput")

    x_all = ext("x_all", [2048, 2048])
    c_fm = ext("c_fm", [128, 16])
    consts = ext("consts", [128, NCONST])
    kidx = ext("kidx", [128, 2048])
    relrow = ext("relrow", [32, 2304])
    relb = ext("rel_bias", [32, 16])
    gv = ext("gv", [128, 3 * 16])
    gsm = ext("gsm", [128, 4])
    w_mod0 = ext("w_mod0", [2048, 12288])
    b_mod0 = ext("b_mod0", [1, 12288])
    kv_w_mod = ext("kv_w_mod", [2048, 4096])
    kv_b_mod = ext("kv_b_mod", [1, 4096])
    a_w_in = ext("a_w_in", [2048, 4176])
    a_w_out = ext("a_w_out", [2048, 2048])
    ffn_w1 = ext("ffn_w1", [2048, 5632])
    ffn_w3 = ext("ffn_w3", [2048, 5632])
    ffn_w2 = ext("ffn_w2", [5632, 2048])
    kv_w = ext("kv_w", [2048, 6144])

    okind = "ExternalOutput" if ctx is None else None
    x1_out = S.dram("x1_out", [128, 16 * 1024], F32, kind=okind)
    kT_out = S.dram("kT_out", [128, 24 * 1024], BF16, kind=okind)
    v_out = S.dram("v_out", [1024, 3072], BF16, kind=okind)
    if ctx is not None:
        ctx["x1_out"], ctx["kT_out"], ctx["v_out"] = x1_out, kT_out, v_out
        KT_all = S.dram("KT_all", [128, 24 * 2048], BF16)
        V_all = S.dram("V_all", [2048, 3072], BF16)
        ctx["KT_all"], ctx["V_all"] = KT_all, V_all
        KT_allv = KT_all[:, :].rearrange("p (a b) -> p a b", a=24)
    frowD = S.dram("frowD", [2 * 16, 2304], BF16)
    oT_d = S.dram("oT_d", [128, 16 * 1024], BF16)

    def dbg_out(name, shape, dt=F32):
        b = S.dram("dbg_" + name, shape, dt, kind="ExternalOutput")
        dbg[name] = b
        return b

    SC.push()
    cf = S.sb("cf", [128, NCONST], F32)
    identb = S.sb("identb", [128, 128], BF16)
    antib = S.sb("antib", [128, 128], BF16)
    identf = cf
    bdones = S.sb("bdones", [128, 128], BF16)
    onesb = S.sb("onesb", [128, 128], BF16)
    cs = S.sb("cs", [128, 16], F32)
    mod0 = S.sb("mod0", [128, 96], F32)
    kvm = S.sb("kvm", [128, 32], F32)
    gvs = S.sb("gvs", [128, 48], F32)
    gs = S.sb("gs", [128, 4], F32)
    s1 = S.sb("s1", [128, 16], F32)
    s2 = S.sb("s2", [128, 16], F32)
    skv = S.sb("skv", [128, 16], F32)
    gq = S.sb("gq", [128, 1], F32)
    S.dma("sp", cf[:, :], consts[:, :], r=[consts], w=[cf])
    S.dma("pool", identb[:, :], consts[:, 0:128], r=[consts], w=[identb])
    S.dma("pool", bdones[:, :], consts[:, 128:256], r=[consts], w=[bdones])
    S.dma("pool", antib[:, :], consts[:, 272:400], r=[consts], w=[antib])
    S.dma("sp", cs[:, :], c_fm[:, :], r=[c_fm], w=[cs])
    S.dma("sp", gvs[:, :], gv[:, :], r=[gv], w=[gvs])
    S.dma("sp", gs[:, :], gsm[:, :], r=[gsm], w=[gs])
    S.v("dve", "memset", onesb[:, :], 1.0, w=[onesb])
    S.act(cs[:, :], cs[:, :], AF.Silu, r=[cs], w=[cs])
    mod_rows(S, SC, cs, w_mod0, b_mod0, 12288, mod0, cf, "m0")
    mod_rows(S, SC, cs, kv_w_mod, kv_b_mod, 4096, kvm, cf, "mk")
    S.v("dve", "scalar_tensor_tensor", s1[:, :], mod0[:, 16:32], 1.0, gvs[:, 0:16], ALU.add, ALU.mult,
        r=[mod0, gvs], w=[s1])
    S.v("dve", "scalar_tensor_tensor", s2[:, :], mod0[:, 64:80], 1.0, gvs[:, 16:32], ALU.add, ALU.mult,
        r=[mod0, gvs], w=[s2])
    S.v("dve", "scalar_tensor_tensor", skv[:, :], kvm[:, 16:32], 1.0, gvs[:, 32:48], ALU.add, ALU.mult,
        r=[kvm, gvs], w=[skv])
    S.v("dve", "tensor_scalar", gq[:, :], gs[:, 0:1], 128.0 ** -0.5, None, ALU.mult, r=[gs], w=[gq])
    epsc = cf[:, 269:270]
    sh1 = mod0[:, 0:16]
    gt1 = mod0[:, 32:48]
    sh2 = mod0[:, 48:64]
    gt2 = mod0[:, 80:96]
    if "mod0" in debug:
        d_ = dbg_out("mod0", [128, 96])
        S.dma("sp", d_[:, :], mod0[:, :], r=[mod0], w=[d_])
        d_ = dbg_out("kvm", [128, 32])
        S.dma("sp", d_[:, :], kvm[:, :], r=[kvm], w=[d_])
    if stop_after == "mod":
        SC.pop()
        return nc, dbg

    SC.push()
    qT = S.sb("qT", [128, 16, 1024], BF16)
    kT = S.sb("kT", [128, 4, 2048], BF16)
    Vs = S.sb("Vs", [128, 16, 512], BF16)
    qiT = S.sb("qiT", [128, 8, 1024], BF16)
    kiT2 = S.sb("kiT2", [128, 2048], BF16)
    wis = S.sb("wis", [128, 8, 16], F32)
    MOFF = [0]
    for j in range(8):
        MOFF.append(MOFF[-1] + 2 * (j + 1) * 128)
    mneg = S.sb("mneg", [128, MOFF[-1]], BF16)

    SC.push()
    hT = S.sb("hT", [128, 16, 2048], BF16)
    SC.push()
    xt = [S.sb(f"xt{i}", [128, 2048], F32) for i in range(2)]
    xn = [S.sb(f"xn{i}", [128, 2048], BF16) for i in range(2)]
    junk = S.sb("junk", [128, 2048], BF16)
    ss = S.sb("ss", [128, 16], F32)
    rs = S.sb("rs", [128, 16], F32)
    ptr = [S.ps(f"ptr{i}", [128, 8, 128], BF16) for i in range(4)]
    n = 0
    for tb in range(16):
        x_ = xt[tb % 2]
        n_ = xn[tb % 2]
        S.dma(WQ[tb % 2], x_[:, :], x_all[tb * 128:(tb + 1) * 128, :], r=[x_all], w=[x_])
        S.act(junk[:, :], x_[:, :], AF.Square, accum_out=ss[:, tb:tb + 1], r=[x_], w=[junk, ss.k(tb)])
        S.act(rs[:, tb:tb + 1], ss[:, tb:tb + 1], AF.Sqrt, scale=1.0 / 2048, bias=epsc,
              r=[ss.k(tb)], w=[rs.k(tb)])
        S.v("dve", "reciprocal", rs[:, tb:tb + 1], rs[:, tb:tb + 1], r=[rs.k(tb)], w=[rs.k(tb)])
        S.v("dve", "tensor_scalar", n_[:, :], x_[:, :], rs[:, tb:tb + 1], None, ALU.mult,
            r=[x_, rs.k(tb)], w=[n_])
        for g in range(2):
            p_ = ptr[n % 4]
            n += 1
            for c8 in range(8):
                c = g * 8 + c8
                S.tr(p_[:, c8, :], n_[:, c * 128:(c + 1) * 128], identb[:, :], r=[n_, identb], w=[p_])
            for c8 in range(8):
                c = g * 8 + c8
                if c8 % 2 == 0:
                    S.act(hT[:, c, tb * 128:(tb + 1) * 128], p_[:, c8, :], AF.Identity,
                          bias=sh1[:, c:c + 1], scale=s1[:, c:c + 1], r=[p_, mod0, s1], w=[hT.k(tb)])
                else:
                    S.v("dve", "tensor_scalar", hT[:, c, tb * 128:(tb + 1) * 128], p_[:, c8, :],
                        s1[:, c:c + 1], sh1[:, c:c + 1], ALU.mult, ALU.add, r=[p_, mod0, s1], w=[hT.k(tb)])
    if "hT" in debug:
        d_ = dbg_out("hT", [128, 16 * 2048], BF16)
        S.dma("sp", d_[:, :].rearrange("p (a b) -> p a b", a=16), hT[:, :, :], r=[hT.k(tb) for tb in range(16)], w=[d_])
    SC.pop()
    if stop_after == "A1":
        SC.pop(); SC.pop(); SC.pop()
        return nc, dbg

    SC.push()
    wt_ = [S.sb(f"w{i}", [128, 16, 256], BF16) for i in range(3)]
    sqb = [S.sb(f"sqb{i}", [128, 512], BF16) for i in range(2)]
    rsd = [S.sb(f"rsd{i}", [128, 512], F32) for i in range(2)]
    pp = [S.ps(f"pp{i}", [128, 512], F32) for i in range(3)]
    pn = [S.ps(f"pn{i}", [128, 512], F32) for i in range(2)]
    hall = [hT.k(tb) for tb in range(16)]
    cnt = {"w": 0, "p": 0, "n": 0}

    def next_w():
        t = wt_[cnt["w"] % 3]
        cnt["w"] += 1
        return t

    def headnorm(p_, ones_, gcol, out_ap, out_buf):
        i = cnt["n"]
        cnt["n"] += 1
        q_ = sqb[i % 2]
        r_ = rsd[i % 2]
        n_ = pn[i % 2]
        S.act(q_[:, :], p_[:, :], AF.Square, r=[p_], w=[q_])
        S.mm(n_[:, :], ones_[:, :], q_[:, :], r=[ones_, q_], w=[n_])
        return q_, r_, n_

    def proj_fm(cols0, ncols_per, gcol, ones_, nrm_div, dst_fn, ntok):
        pass

    def fm_heads(col0, nheads, ntok, dstbuf, gcol, normalize, ones_, div):
        for hp in range(nheads // 2):
            w_ = next_w()
            wload(S, "pool", w_, w_[:, :, :], a_w_in, col0 + hp * 256, 256)
            for hh in range(2):
                h = hp * 2 + hh
                for q4 in range(ntok // 512):
                    p_ = pp[cnt["p"] % 3]
                    cnt["p"] += 1
                    ts_ = slice(q4 * 512, (q4 + 1) * 512)
                    for kc in range(16):
                        S.mm(p_[:, :], w_[:, kc, hh * 128:(hh + 1) * 128], hT[:, kc, ts_],
                             start=(kc == 0), stop=(kc == 15), r=[w_] + hall, w=[p_])
                    if normalize:
                        i = cnt["n"]
                        cnt["n"] += 1
                        q_ = sqb[i % 2]
                        r_ = rsd[i % 2]
                        n_ = pn[i % 2]
                        S.act(q_[:, :], p_[:, :], AF.Square, r=[p_], w=[q_])
                        S.mm(n_[:, :], ones_[:, :], q_[:, :], r=[ones_, q_], w=[n_])
                        S.act(r_[:, :], n_[:, :], AF.Sqrt, scale=1.0 / div, bias=epsc, r=[n_], w=[r_])
                        S.v("dve", "reciprocal", r_[:, :], r_[:, :], r=[r_], w=[r_])
                        S.v("dve", "scalar_tensor_tensor", dstbuf[:, h, ts_], p_[:, :], gcol, r_[:, :],
                            ALU.mult, ALU.mult, r=[p_, r_, gs, gq], w=[dstbuf])
                    else:
                        S.act(dstbuf[:, h, ts_], p_[:, :], AF.Copy, r=[p_], w=[dstbuf])

    fm_heads(0, 16, 1024, qT, gq[:, 0:1], True, onesb, 128.0)
    fm_heads(2048, 4, 2048, kT, gs[:, 1:2], True, onesb, 128.0)
    fm_heads(3072, 8, 1024, qiT, None, False, None, None)
    w_ = next_w()
    S.dma("pool", w_[:, :, 0:64], a_w_in[:, 4096:4160].rearrange("(k p) n -> p k n", p=128), r=[a_w_in], w=[w_])
    S.dma("pool", w_[:, :, 64:128], a_w_in[:, 4096:4160].rearrange("(k p) n -> p k n", p=128), r=[a_w_in], w=[w_])
    S.dma("pool", w_[:, :, 128:144], a_w_in[:, 4160:4176].rearrange("(k p) n -> p k n", p=128), r=[a_w_in], w=[w_])
    for q4 in range(4):
        p_ = pp[cnt["p"] % 3]
        cnt["p"] += 1
        ts_ = slice(q4 * 512, (q4 + 1) * 512)
        for kc in range(16):
            S.mm(p_[:, :], w_[:, kc, 0:128], hT[:, kc, ts_], start=(kc == 0), stop=(kc == 15),
                 r=[w_] + hall, w=[p_])
        S.act(kiT2[:, ts_], p_[:, :], AF.Copy, r=[p_], w=[kiT2])
    for tb in range(8):
        p_ = pp[cnt["p"] % 3]
        cnt["p"] += 1
        for kc in range(16):
            S.mm(p_[:, 0:16], hT[:, kc, tb * 128:(tb + 1) * 128], w_[:, kc, 128:144],
                 start=(kc == 0), stop=(kc == 15), r=[w_] + hall, w=[p_])
        S.v("dve", "tensor_scalar", wis[:, tb, :], p_[:, 0:16], (64.0 ** -0.5) * (16.0 ** -0.5), None, ALU.mult,
            r=[p_], w=[wis])
    for hp in range(2):
        w_ = next_w()
        wload(S, "pool", w_, w_[:, :, :], a_w_in, 2560 + hp * 256, 256)
        for tb in range(16):
            p_ = pp[cnt["p"] % 3]
            cnt["p"] += 1
            for kc in range(16):
                S.mm(p_[:, 0:256], hT[:, kc, tb * 128:(tb + 1) * 128], w_[:, kc, :],
                     start=(kc == 0), stop=(kc == 15), r=[w_] + hall, w=[p_])
            if tb % 2 == 0:
                S.act(Vs[:, tb, hp * 256:(hp + 1) * 256], p_[:, 0:256], AF.Copy, r=[p_], w=[Vs])
            else:
                S.v("dve", "tensor_copy", Vs[:, tb, hp * 256:(hp + 1) * 256], p_[:, 0:256], r=[p_], w=[Vs])
    for nm, b_, shp in (("qT", qT, [128, 16 * 1024]), ("kT", kT, [128, 4 * 2048]), ("Vs", Vs, [128, 16 * 512]),
                        ("qiT", qiT, [128, 8 * 1024]), ("kiT2", kiT2, [128, 2048])):
        if nm in debug:
            d_ = dbg_out(nm, shp, BF16)
            if nm == "kiT2":
                S.dma("sp", d_[:, :], b_[:, :], r=[b_], w=[d_])
            else:
                S.dma("sp", d_[:, :].rearrange("p (a b) -> p a b", a=b_.t.shape[1]), b_[:, :, :], r=[b_], w=[d_])
    if "wis" in debug:
        d_ = dbg_out("wis", [128, 128])
        S.dma("sp", d_[:, :].rearrange("p (a b) -> p a b", a=8), wis[:, :, :], r=[wis], w=[d_])
    SC.pop()
    SC.pop()
    if stop_after == "A2":
        SC.pop(); SC.pop()
        return nc, dbg
    SC.push()
    kidxb = S.sb("kidxb", [128, 2048], F32)
    S.dma("sp", kidxb[:, :], kidx[:, :], r=[kidx], w=[kidxb])
    isc = [S.sb(f"isc{i}", [128, 2048], F32) for i in range(2)]
    work = S.sb("work", [128, 2048], F32)
    rr = [S.sb(f"rr{i}", [128, 512], F32) for i in range(3)]
    m8 = [S.sb(f"m8{i}", [128, 8], F32) for i in range(2)]
    pen = [S.sb(f"pen{i}", [128, 128], F32) for i in range(2)]
    pi = [S.ps(f"pi{i}", [128, 512], F32) for i in range(3)]
    npi = 0
    for j in range(8):
        W_ = (j + 1) * 128
        L = 2 * W_
        ic = isc[j % 2]
        for rng in range(2):
            for p0 in range(0, W_, 512):
                pw = min(512, W_ - p0)
                sc0 = rng * 1024 + p0
                ic0 = rng * W_ + p0
                for h in range(16):
                    pr_, hf = h // 2, h % 2
                    ps_ = slice(64 * hf, 64 * hf + 64)
                    p_ = pi[npi % 3]
                    r_ = rr[npi % 3]
                    npi += 1
                    S.mm(p_[:, 0:pw], qiT[ps_, pr_, j * 128:(j + 1) * 128], kiT2[ps_, sc0:sc0 + pw], w=[p_])
                    S.act(r_[:, 0:pw], p_[:, 0:pw], AF.Relu, r=[p_], w=[r_])
                    if h == 0:
                        S.v("dve", "tensor_scalar", ic[:, ic0:ic0 + pw], r_[:, 0:pw], wis[:, j, 0:1], None, ALU.mult,
                            r=[r_], w=[ic])
                    else:
                        S.v("dve", "scalar_tensor_tensor", ic[:, ic0:ic0 + pw], r_[:, 0:pw], wis[:, j, h:h + 1],
                            ic[:, ic0:ic0 + pw], ALU.mult, ALU.add, r=[r_, ic], w=[ic])
        for rng in range(2):
            pn_ = pen[rng]
            sc0 = rng * 1024 + j * 128
            ic0 = rng * W_ + j * 128
            S.v("dve", "tensor_scalar", pn_[:, :], kidxb[:, sc0:sc0 + 128], cf[:, 256 + j:257 + j], BIGNEG,
                ALU.is_gt, ALU.mult, r=[kidxb], w=[pn_])
            S.v("dve", "tensor_tensor", ic[:, ic0:ic0 + 128], ic[:, ic0:ic0 + 128], pn_[:, :], ALU.add,
                r=[ic, pn_], w=[ic])
        if "isc" in debug and j == 3:
            d_ = dbg_out("isc", [128, 2048])
            S.dma("sp", d_[:, :], ic[:, :], r=[ic], w=[d_])
        mo = MOFF[j]
        if j == 0:
            S.v("dve", "tensor_scalar", mneg[:, mo:mo + L], ic[:, 0:L], -1.0e29, NEG, ALU.is_lt, ALU.mult,
                r=[ic], w=[mneg])
        else:
            mm_ = m8[0]
            S.v("dve", "max", mm_[:, :], ic[:, 0:L], r=[ic], w=[mm_])
            S.v("dve", "match_replace", work[:, 0:L], mm_[:, :], ic[:, 0:L], BIGNEG, r=[ic, mm_], w=[work])
            for rd in range(1, 32):
                mm_ = m8[rd % 2]
                S.v("dve", "max", mm_[:, :], work[:, 0:L], r=[work], w=[mm_])
                if rd < 31:
                    S.v("dve", "match_replace", work[:, 0:L], mm_[:, :], work[:, 0:L], BIGNEG, r=[work, mm_], w=[work])
            S.v("dve", "tensor_scalar", mneg[:, mo:mo + L], ic[:, 0:L], mm_[:, 7:8], NEG, ALU.is_lt, ALU.mult,
                r=[ic, mm_], w=[mneg])
    if "mneg" in debug:
        d_ = dbg_out("mneg", [128, MOFF[-1]], BF16)
        S.dma("sp", d_[:, :], mneg[:, :], r=[mneg], w=[d_])
    SC.pop()
    if stop_after == "idx":
        SC.pop(); SC.pop()
        return nc, dbg

    SC.push()
    Gt = [S.sb("Gown", [128, 16, 8, 128], BF16), S.sb("Gpar", [128, 16, 8, 128], BF16)]
    SC.push()
    relr = S.sb("relr", [32, 2304], F32)
    nn_ = S.sb("nn_", [32, 2304], F32)
    aa_ = S.sb("aa_", [32, 2304], F32)
    relbs = S.sb("relbs", [32, 16], F32)
    frow = S.sb("frow", [16, 2304], BF16)
    pfr = [S.ps(f"pfr{i}", [16, 512], F32) for i in range(2)]
    S.dma("sp", relr[:, :], relrow[:, :], r=[relrow], w=[relr])
    S.dma("sp", relbs[:, :], relb[:, :], r=[relb], w=[relbs])
    for kind in range(2):
        if kind == 0:
            S.v("dve", "tensor_scalar", nn_[:, :], relr[:, :], 0.0, None, ALU.max, r=[relr], w=[nn_])
        else:
            S.v("dve", "tensor_scalar", nn_[:, :], relr[:, :], cf[0:32, 264:265], 0.0, ALU.add, ALU.max,
                r=[relr], w=[nn_])
        S.v("dve", "tensor_scalar", aa_[:, :], nn_[:, :], cf[0:32, 265:266], None, ALU.is_ge, r=[nn_], w=[aa_])
        S.v("dve", "scalar_tensor_tensor", aa_[:, :], nn_[:, :], cf[0:32, 266:267], aa_[:, :], ALU.is_lt, ALU.mult,
            r=[nn_, aa_], w=[aa_])
        for pc in range(5):
            c0 = pc * 512
            pw = min(512, 2304 - c0)
            p_ = pfr[pc % 2]
            S.mm(p_[:, 0:pw], relbs[:, :], aa_[:, c0:c0 + pw], r=[relbs, aa_], w=[p_])
            S.act(frow[:, c0:c0 + pw], p_[:, 0:pw], AF.Copy, r=[p_], w=[frow])
        S.dma("sp", frowD[kind * 16:(kind + 1) * 16, :], frow[:, :], r=[frow], w=[frowD])
    for kind in range(2):
        for h in range(16):
            src = bass.AP(frowD.t.tensor, (kind * 16 + h) * 2304 + 129, [[1, 128], [256, 8], [1, 128]])
            S.dma(WQ[h % 2], Gt[kind][:, h, :, :], src, r=[frowD], w=[Gt[kind]])
    SC.pop()
    if "G" in debug:
        d_ = dbg_out("G", [128, 2 * 16 * 8 * 128], BF16)
        for kind in range(2):
            S.dma("sp", d_[:, kind * 16384:(kind + 1) * 16384].rearrange("p (h t q) -> p h t q", h=16, t=8),
                  Gt[kind][:, :, :, :], r=[Gt[kind]], w=[d_])

    SC.push()
    pT = [S.sb(f"pT{i}", [128, 4, 128], BF16) for i in range(3)]
    rden = [S.sb(f"rden{i}", [128, 128], F32) for i in range(2)]
    ostage = [S.sb(f"ostage{i}", [128, 16, 128], BF16) for i in range(2)]
    st = [S.ps(f"st{i}", [128, 4, 128], F32) for i in range(3)]
    po = [S.ps(f"po{i}", [128, 512], F32) for i in range(2)]
    pd = [S.ps(f"pd{i}", [128, 512], F32) for i in range(2)]
    oT_dv = oT_d[:, :].rearrange("p (h t) -> p h t", h=16)
    nst = 0
    nh = 0
    for j in range(8):
        blocks = [(0, m) for m in range(j + 1)] + [(1, m) for m in range(j + 1)]
        nblk = len(blocks)
        og = ostage[j % 2]
        for h in range(16):
            kvh = h // 4
            po_ = po[nh % 2]
            pd_ = pd[nh % 2]
            rd_ = rden[nh % 2]
            nh += 1
            for g0 in range(0, nblk, 4):
                grp = blocks[g0:g0 + 4]
                st_ = st[nst % 3]
                pT_ = pT[nst % 3]
                nst += 1
                for ii, (kind, m) in enumerate(grp):
                    i = g0 + ii
                    sb_ = kind * 8 + m
                    S.mm(st_[:, ii, :], kT[:, kvh, sb_ * 128:(sb_ + 1) * 128], qT[:, h, j * 128:(j + 1) * 128],
                         start=True, stop=False, w=[st_])
                    S.mm(st_[:, ii, :], mneg[:, MOFF[j] + i * 128:MOFF[j] + (i + 1) * 128], identb[:, :],
                         start=False, stop=False, w=[st_])
                    S.mm(st_[:, ii, :], antib[:, :], Gt[kind][:, h, j - m, :], start=False, stop=True,
                         r=[Gt[kind]], w=[st_])
                ng = len(grp)
                S.act(pT_[:, 0:ng, :], st_[:, 0:ng, :], AF.Exp, r=[st_], w=[pT_])
                for ii, (kind, m) in enumerate(grp):
                    i = g0 + ii
                    sb_ = kind * 8 + m
                    S.mm(po_[:, 0:128], Vs[:, sb_, kvh * 128:(kvh + 1) * 128], pT_[:, ii, :],
                         start=(i == 0), stop=(i == nblk - 1), r=[pT_], w=[po_], sig=False)
                    S.mm(pd_[:, 0:128], onesb[:, :], pT_[:, ii, :],
                         start=(i == 0), stop=(i == nblk - 1), r=[pT_], w=[pd_], sig=(ii == ng - 1))
            S.v("dve", "reciprocal", rd_[:, :], pd_[:, 0:128], r=[pd_], w=[rd_])
            S.v("dve", "tensor_tensor", og[:, h, :], po_[:, 0:128], rd_[:, :], ALU.mult, r=[po_, pd_, rd_], w=[og])
        S.dma("sp", oT_dv[:, :, j * 128:(j + 1) * 128], og[:, :, :], r=[og], w=[oT_d])
    SC.pop()
    SC.pop()
    SC.pop()
    if stop_after == "att":
        if "oT" in debug:
            SC.push()
            tmpo = S.sb("tmpo", [128, 16 * 1024], BF16)
            d_ = dbg_out("oT", [128, 16 * 1024], BF16)
            S.dma("sp", tmpo[:, :], oT_d[:, :], r=[oT_d], w=[tmpo])
            S.dma("sp", d_[:, :], tmpo[:, :], r=[tmpo], w=[d_])
            SC.pop()
        SC.pop()
        return nc, dbg
    SC.push()
    xT = S.sb("xT", [128, 16, 1024], F32)
    h2T = S.sb("h2T", [128, 16, 1024], BF16)
    rstd = S.sb("rstd", [128, 1024], F32)
    SC.push()
    oT = S.sb("oT", [128, 16, 1024], BF16)
    wt_ = [S.sb(f"wo{i}", [128, 16, 256], BF16) for i in range(2)]
    xt = [S.sb(f"xtb{i}", [128, 2048], F32) for i in range(2)]
    ptx = [S.ps(f"ptx{i}", [128, 4, 128], F32) for i in range(2)]
    pa = [S.ps(f"pa{i}", [128, 512], F32) for i in range(3)]
    S.dma("sp", oT[:, :, :], oT_d[:, :].rearrange("p (h t) -> p h t", h=16), r=[oT_d], w=[oT])
    n = 0
    for tb in range(8):
        x_ = xt[tb % 2]
        S.dma(WQ[tb % 2], x_[:, :], x_all[tb * 128:(tb + 1) * 128, :], r=[x_all], w=[x_])
        for c4 in range(4):
            p_ = ptx[n % 2]
            n += 1
            for ci in range(4):
                c = c4 * 4 + ci
                S.tr(p_[:, ci, :], x_[:, c * 128:(c + 1) * 128], cf[:, 0:128], r=[x_], w=[p_])
            if c4 % 2 == 0:
                S.v("dve", "tensor_copy", xT[:, c4 * 4:c4 * 4 + 4, tb * 128:(tb + 1) * 128], p_[:, :, :], r=[p_], w=[xT.k(tb // 4)])
            else:
                S.act(xT[:, c4 * 4:c4 * 4 + 4, tb * 128:(tb + 1) * 128], p_[:, :, :], AF.Copy, r=[p_], w=[xT.k(tb // 4)])
    n = 0
    for fcg in range(8):
        w_ = wt_[fcg % 2]
        wload(S, "pool", w_, w_[:, :, :], a_w_out, fcg * 256, 256)
        for fc2 in range(2):
            fc = fcg * 2 + fc2
            for half in range(2):
                p_ = pa[n % 3]
                n += 1
                hs = slice(half * 512, (half + 1) * 512)
                for h in range(16):
                    S.mm(p_[:, :], w_[:, h, fc2 * 128:(fc2 + 1) * 128], oT[:, h, hs], start=(h == 0), stop=(h == 15),
                         r=[w_, oT], w=[p_])
                S.v("dve", "scalar_tensor_tensor", xT[:, fc, hs], p_[:, :], gt1[:, fc:fc + 1], xT[:, fc, hs],
                    ALU.mult, ALU.add, r=[p_, xT.k(half)], w=[xT.k(half)])
    SC.pop()
    if "xa" in debug:
        d_ = dbg_out("xa", [128, 16 * 1024])
        S.dma("sp", d_[:, :].rearrange("p (a b) -> p a b", a=16), xT[:, :, :], r=[xT.k(0), xT.k(1)], w=[d_])
    if stop_after == "wout":
        SC.pop(); SC.pop()
        return nc, dbg

    def norm_phase(scol, bcol, tag=[0]):
        SC.push()
        tag[0] += 1
        sq = [S.sb(f"sq{i}_{tag[0]}", [128, 512], BF16) for i in range(2)]
        tmp = [S.sb(f"tmpn{i}_{tag[0]}", [128, 512], F32) for i in range(2)]
        pss = [S.ps(f"pss{i}_{tag[0]}", [128, 512], F32) for i in range(2)]
        xall = [xT.k(0), xT.k(1)]
        for half in range(2):
            cs_ = slice(half * 512, (half + 1) * 512)
            ps_ = pss[half]
            for c in range(16):
                q = sq[c % 2]
                S.act(q[:, :], xT[:, c, cs_], AF.Square, r=xall, w=[q])
                S.mm(ps_[:, :], onesb[:, :], q[:, :], start=(c == 0), stop=(c == 15), r=[q], w=[ps_])
            S.act(rstd[:, cs_], ps_[:, :], AF.Sqrt, scale=1.0 / 2048, bias=epsc, r=[ps_], w=[rstd])
            S.v("dve", "reciprocal", rstd[:, cs_], rstd[:, cs_], r=[rstd], w=[rstd])
            for c in range(16):
                t = tmp[c % 2]
                S.v("dve", "scalar_tensor_tensor", t[:, :], xT[:, c, cs_], scol[:, c:c + 1], rstd[:, cs_],
                    ALU.mult, ALU.mult, r=xall + [rstd], w=[t])
                S.act(h2T[:, c, cs_], t[:, :], AF.Identity, bias=bcol[:, c:c + 1], scale=1.0, r=[t], w=[h2T])
        SC.pop()

    norm_phase(s2, sh2)
    if "h2" in debug:
        d_ = dbg_out("h2", [128, 16 * 1024], BF16)
        S.dma("sp", d_[:, :].rearrange("p (a b) -> p a b", a=16), h2T[:, :, :], r=[h2T], w=[d_])
    if stop_after == "norm2":
        SC.pop(); SC.pop()
        return nc, dbg
    SC.push()
    w1g = [S.sb(f"w1g{i}", [128, 16, 256], BF16) for i in range(2)]
    w3g = [S.sb(f"w3g{i}", [128, 16, 256], BF16) for i in range(2)]
    w2g = [S.sb(f"w2g{i}", [128, 2, 2048], BF16) for i in range(2)]
    ug = [S.sb(f"ug{i}", [128, 2, 1024], BF16) for i in range(2)]
    sil = [S.sb(f"sil{i}", [128, 512], F32) for i in range(2)]
    p1 = [S.ps(f"p1{i}", [128, 512], F32) for i in range(2)]
    p3 = [S.ps(f"p3{i}", [128, 512], F32) for i in range(2)]
    py = [S.ps(f"py{i}", [128, 512], F32) for i in range(3)]
    n13 = 0
    ny = 0
    for g in range(22):
        a1, a3, a2, u_ = w1g[g % 2], w3g[g % 2], w2g[g % 2], ug[g % 2]
        wload(S, "pool", a1, a1[:, :, :], ffn_w1, g * 256, 256)
        wload(S, "pool", a3, a3[:, :, :], ffn_w3, g * 256, 256)
        for c in range(2):
            for hh in range(2):
                S.dma("pool", a2[:, c, hh * 1024:(hh + 1) * 1024],
                      ffn_w2[g * 256 + c * 128:g * 256 + (c + 1) * 128, hh * 1024:(hh + 1) * 1024],
                      r=[ffn_w2], w=[a2])
        for c in range(2):
            for half in range(2):
                hs = slice(half * 512, (half + 1) * 512)
                q1, q3, sl = p1[n13 % 2], p3[n13 % 2], sil[n13 % 2]
                n13 += 1
                for kc in range(16):
                    S.mm(q1[:, :], a1[:, kc, c * 128:(c + 1) * 128], h2T[:, kc, hs], start=(kc == 0), stop=(kc == 15),
                         r=[a1, h2T], w=[q1])
                for kc in range(16):
                    S.mm(q3[:, :], a3[:, kc, c * 128:(c + 1) * 128], h2T[:, kc, hs], start=(kc == 0), stop=(kc == 15),
                         r=[a3, h2T], w=[q3])
                S.act(sl[:, :], q1[:, :], AF.Silu, r=[q1], w=[sl])
                S.v("dve", "tensor_tensor", u_[:, c, hs], sl[:, :], q3[:, :], ALU.mult, r=[sl, q3], w=[u_])
        for fc in range(16):
            for half in range(2):
                hs = slice(half * 512, (half + 1) * 512)
                y_ = py[ny % 3]
                ny += 1
                for c in range(2):
                    S.mm(y_[:, :], a2[:, c, fc * 128:(fc + 1) * 128], u_[:, c, hs], start=(c == 0), stop=(c == 1),
                         r=[a2, u_], w=[y_])
                S.v("dve", "scalar_tensor_tensor", xT[:, fc, hs], y_[:, :], gt2[:, fc:fc + 1], xT[:, fc, hs],
                    ALU.mult, ALU.add, r=[y_, xT.k(half)], w=[xT.k(half)])
    SC.pop()
    S.dma("sp", x1_out[:, :].rearrange("p (a b) -> p a b", a=16), xT[:, :, :], r=[xT.k(0), xT.k(1)], w=[x1_out])
    if stop_after == "ffn":
        SC.pop(); SC.pop()
        return nc, dbg

    norm_phase(skv, kvm)
    SC.push()
    wk_ = [S.sb(f"wk{i}", [128, 16, 256], BF16) for i in range(2)]
    sqb = [S.sb(f"ksq{i}", [128, 512], BF16) for i in range(2)]
    rsd = [S.sb(f"krs{i}", [128, 512], F32) for i in range(2)]
    kt_ = [S.sb(f"kt{i}", [128, 512], BF16) for i in range(3)]
    vt_ = [S.sb(f"vt{i}", [128, 256], BF16) for i in range(3)]
    pk = [S.ps(f"pk{i}", [128, 512], F32) for i in range(3)]
    pn = [S.ps(f"pkn{i}", [128, 512], F32) for i in range(2)]
    kT_ov = kT_out[:, :].rearrange("p (a b) -> p a b", a=24)
    n = 0
    for g2 in range(12):
        w_ = wk_[g2 % 2]
        wload(S, "pool", w_, w_[:, :, :], kv_w, g2 * 256, 256)
        for c in range(2):
            ptile = g2 * 2 + c
            for half in range(2):
                hs = slice(half * 512, (half + 1) * 512)
                p_, q_, r_, n_, k_ = pk[n % 3], sqb[n % 2], rsd[n % 2], pn[n % 2], kt_[n % 3]
                n += 1
                for kc in range(16):
                    S.mm(p_[:, :], w_[:, kc, c * 128:(c + 1) * 128], h2T[:, kc, hs], start=(kc == 0), stop=(kc == 15),
                         r=[w_, h2T], w=[p_])
                S.act(q_[:, :], p_[:, :], AF.Square, r=[p_], w=[q_])
                S.mm(n_[:, :], bdones[:, :], q_[:, :], r=[q_], w=[n_])
                S.act(r_[:, :], n_[:, :], AF.Sqrt, scale=1.0 / 64, bias=epsc, r=[n_], w=[r_])
                S.v("dve", "reciprocal", r_[:, :], r_[:, :], r=[r_], w=[r_])
                S.v("dve", "scalar_tensor_tensor", k_[:, :], p_[:, :], gs[:, 2:3], r_[:, :], ALU.mult, ALU.mult,
                    r=[p_, r_], w=[k_])
                S.dma("sp", kT_ov[:, ptile, hs], k_[:, :], r=[k_], w=[kT_out])
                if ctx is not None:
                    S.dma("act", KT_allv[:, ptile, hs], k_[:, :], r=[k_], w=[KT_all])
    n = 0
    for g2 in range(12):
        w_ = wk_[g2 % 2]
        wload(S, "pool", w_, w_[:, :, :], kv_w, 3072 + g2 * 256, 256)
        for tb in range(8):
            p_, v_ = pk[n % 3], vt_[n % 3]
            n += 1
            for kc in range(16):
                S.mm(p_[:, 0:256], h2T[:, kc, tb * 128:(tb + 1) * 128], w_[:, kc, :], start=(kc == 0), stop=(kc == 15),
                     r=[w_, h2T], w=[p_])
            if tb % 2 == 0:
                S.act(v_[:, :], p_[:, 0:256], AF.Copy, r=[p_], w=[v_])
            else:
                S.v("dve", "tensor_copy", v_[:, :], p_[:, 0:256], r=[p_], w=[v_])
            S.dma("sp", v_out[tb * 128:(tb + 1) * 128, g2 * 256:(g2 + 1) * 256], v_[:, :], r=[v_], w=[v_out])
            if ctx is not None:
                S.dma("act", V_all[tb * 128:(tb + 1) * 128, g2 * 256:(g2 + 1) * 256], v_[:, :], r=[v_], w=[V_all])
    SC.pop()
    SC.pop()
    SC.pop()
    return nc, dbg


def fm16(v):
    return np.ascontiguousarray(np.asarray(v, np.float32).reshape(16, 128).T)


def prep_A(inp):
    maps = []
    for core in range(8):
        b, par = core // 2, core % 2
        order = storage_order(par)
        xb = inp["x"][b].reshape(16, 128, 2048)[order].reshape(2048, 2048)
        gsm = np.zeros((128, 4), np.float32)
        gsm[:, 0] = inp["a_g_qn"][0]
        gsm[:, 1] = inp["a_g_kn"][0]
        gsm[:, 2] = np.tile(inp["b_g_kn"], 2)
        m = {
            "x_all": np.ascontiguousarray(xb),
            "c_fm": fm16(inp["c"][b]),
            "consts": make_consts(par),
            "kidx": np.ascontiguousarray(np.broadcast_to(make_kidx(par), (128, 2048))),
            "relrow": make_relrow(),
            "rel_bias": np.ascontiguousarray(inp["rel_bias"]),
            "gv": np.concatenate([fm16(inp["g_attn"][0]), fm16(inp["g_ffn"][0]), fm16(inp["kv_g"])], axis=1),
            "gsm": gsm,
            "w_mod0": inp["w_mod"][0], "b_mod0": inp["b_mod"][0][None, :],
            "kv_w_mod": inp["kv_w_mod"], "kv_b_mod": inp["kv_b_mod"][None, :],
            "a_w_in": inp["a_w_in"][0], "a_w_out": inp["a_w_out"][0],
            "ffn_w1": inp["ffn_w1"][0], "ffn_w3": inp["ffn_w3"][0], "ffn_w2": inp["ffn_w2"][0],
            "kv_w": inp["kv_w"],
        }
        maps.append(m)
    return maps


L1_WIN = ((128, 1), (512, 4), (2048, 16))


def l1_tiles():
    TL = []
    for g, (win, r) in enumerate(L1_WIN):
        for kind in range(2):
            for t in range(8):
                ok = False
                for par in range(2):
                    off = 128 * (2 * par - 1) if kind == 1 else 0
                    lo = 256 * t + off - 127
                    hi = 256 * t + off + 127
                    if hi >= 0 and lo <= win:
                        ok = True
                if ok:
                    TL.append((g, kind, t))
    return TL


def make_l1_masks(parity):
    TL = l1_tiles()
    k = np.arange(128)[:, None]
    q = np.arange(128)[None, :]
    m = np.zeros((128, len(TL), 128), np.float32)
    for i, (g, kind, t) in enumerate(TL):
        win, r = L1_WIN[g]
        off = 128 * (2 * parity - 1) if kind == 1 else 0
        rel = 256 * t + off + q - k
        ok = (rel >= 0) & (rel <= win) & (rel % r == 0)
        m[:, i, :] = np.where(ok, 0.0, NEG)
    return np.ascontiguousarray(m.reshape(128, -1))


def build_B(stop_after=None, debug=(), dense_experts=True, ctx=None):
    if ctx is None:
        nc = bass.Bass("TRN2", target_bir_lowering=False)
        S = Sched(nc)
        SC = Scope(S)
    else:
        nc, S, SC = ctx["nc"], ctx["S"], ctx["SC"]
    S.prefix = "zb_"
    dbg = {}
    TL = l1_tiles()
    NT = len(TL)

    def ext(name, shape, dt=F32):
        return S.dram(name, shape, dt, kind="ExternalInput")

    if ctx is None:
        x1fm = ext("x1fm", [128, 16 * 1024])
        KT_all = ext("KT_all", [128, 24 * 2048], BF16)
        V_all = ext("V_all", [2048, 3072], BF16)
    else:
        x1fm, KT_all, V_all = ctx["x1_out"], ctx["KT_all"], ctx["V_all"]
    c_fm = ext("c_fm", [128, 16])
    consts = ext("consts", [128, NCONST])
    relrow = ext("relrow", [32, 2304])
    relb = ext("rel_bias", [32, 16])
    gv = ext("gvb", [128, 32])
    gsm = ext("gsmb", [128, 4])
    maskl1 = ext("maskl1", [128, NT * 128])
    selc = ext("selc", [8, 8 * 128])
    rb_bc = ext("rb_bc", [128, 8])
    w_mod1 = ext("w_mod1", [2048, 12288])
    b_mod1 = ext("b_mod1", [1, 12288])
    b_w_q = ext("b_w_q", [2048, 3072])
    b_w_out = ext("b_w_out", [1024, 2048])
    router = ext("router", [2048, 8])
    moe_w1 = ext("moe_w1", [8 * 2048, 7168])
    moe_w3 = ext("moe_w3", [8 * 2048, 7168])
    moe_w2 = ext("moe_w2", [8 * 7168, 2048])
    out_fm = S.dram("out_fm", [128, 16 * 1024], F32, kind="ExternalOutput")
    frowD = S.dram("frowD", [2 * 16, 2304], BF16)
    oT_d = S.dram("oT_d", [64, 16 * 1024], BF16)

    def dbg_out(name, shape, dt=F32):
        b = S.dram("dbg_" + name, shape, dt, kind="ExternalOutput")
        dbg[name] = b
        return b

    SC.push()
    cf = S.sb("cf", [128, NCONST], F32)
    identb = S.sb("identb", [128, 128], BF16)
    antib = S.sb("antib", [128, 128], BF16)
    bdones = S.sb("bdones", [128, 128], BF16)
    onesb = S.sb("onesb", [128, 128], BF16)
    cs = S.sb("cs", [128, 16], F32)
    mod1 = S.sb("mod1", [128, 96], F32)
    gvs = S.sb("gvs", [128, 32], F32)
    gs = S.sb("gs", [128, 4], F32)
    s1 = S.sb("s1", [128, 16], F32)
    s2 = S.sb("s2", [128, 16], F32)
    gq = S.sb("gq", [128, 1], F32)
    S.dma("sp", cf[:, :], consts[:, :], r=[consts], w=[cf])
    S.dma("pool", identb[:, :], consts[:, 0:128], r=[consts], w=[identb])
    S.dma("pool", bdones[:, :], consts[:, 128:256], r=[consts], w=[bdones])
    S.dma("pool", antib[:, :], consts[:, 272:400], r=[consts], w=[antib])
    S.dma("sp", cs[:, :], c_fm[:, :], r=[c_fm], w=[cs])
    S.dma("sp", gvs[:, :], gv[:, :], r=[gv], w=[gvs])
    S.dma("sp", gs[:, :], gsm[:, :], r=[gsm], w=[gs])
    S.v("dve", "memset", onesb[:, :], 1.0, w=[onesb])
    S.act(cs[:, :], cs[:, :], AF.Silu, r=[cs], w=[cs])
    mod_rows(S, SC, cs, w_mod1, b_mod1, 12288, mod1, cf, "m1")
    S.v("dve", "scalar_tensor_tensor", s1[:, :], mod1[:, 16:32], 1.0, gvs[:, 0:16], ALU.add, ALU.mult,
        r=[mod1, gvs], w=[s1])
    S.v("dve", "scalar_tensor_tensor", s2[:, :], mod1[:, 64:80], 1.0, gvs[:, 16:32], ALU.add, ALU.mult,
        r=[mod1, gvs], w=[s2])
    S.v("dve", "tensor_scalar", gq[:, :], gs[:, 0:1], 64.0 ** -0.5, None, ALU.mult, r=[gs], w=[gq])
    epsc = cf[:, 269:270]
    sh1 = mod1[:, 0:16]
    gt1 = mod1[:, 32:48]
    sh2 = mod1[:, 48:64]
    gt2 = mod1[:, 80:96]
    S.flush()

    SC.push()
    qT1 = S.sb("qT1", [128, 24, 1024], BF16)
    SC.push()
    xT = S.sb("xTa", [128, 16, 1024], F32)
    hT = S.sb("hTa", [128, 16, 1024], BF16)
    rstd = S.sb("rstda", [128, 1024], F32)
    S.dma("sp", xT[:, :, :], x1fm[:, :].rearrange("p (a b) -> p a b", a=16), r=[x1fm], w=[xT])

    def norm_phase(xT_, h_out, rstd_, scol, bcol, tagn, router_w=None, plog=None):
        SC.push()
        sq = [S.sb(f"sq{i}_{tagn}", [128, 512], BF16) for i in range(2)]
        tmp = [S.sb(f"tmpn{i}_{tagn}", [128, 512], F32) for i in range(2)]
        pss = [S.ps(f"pss{i}_{tagn}", [128, 512], F32) for i in range(2)]
        for half in range(2):
            cs_ = slice(half * 512, (half + 1) * 512)
            ps_ = pss[half]
            for c in range(16):
                q = sq[c % 2]
                S.act(q[:, :], xT_[:, c, cs_], AF.Square, r=[xT_], w=[q])
                S.mm(ps_[:, :], onesb[:, :], q[:, :], start=(c == 0), stop=(c == 15), r=[q], w=[ps_])
            S.act(rstd_[:, cs_], ps_[:, :], AF.Sqrt, scale=1.0 / 2048, bias=epsc, r=[ps_], w=[rstd_])
            S.v("dve", "reciprocal", rstd_[:, cs_], rstd_[:, cs_], r=[rstd_], w=[rstd_])
            for c in range(16):
                t = tmp[c % 2]
                S.v("dve", "scalar_tensor_tensor", t[:, :], xT_[:, c, cs_], scol[:, c:c + 1], rstd_[:, cs_],
                    ALU.mult, ALU.mult, r=[xT_, rstd_], w=[t])
                if router_w is None:
                    S.act(h_out[:, c, cs_], t[:, :], AF.Identity, bias=bcol[:, c:c + 1], scale=1.0, r=[t], w=[h_out])
                else:
                    S.v("dve", "tensor_scalar", t[:, :], t[:, :], bcol[:, c:c + 1], None, ALU.add, r=[t], w=[t])
                    S.act(h_out[:, c, cs_], t[:, :], AF.Copy, r=[t], w=[h_out])
                    for b4 in range(4):
                        pl = plog[half * 4 + b4]
                        S.mm(pl[:, 0:8], t[:, b4 * 128:(b4 + 1) * 128], router_w[:, c, :], start=(c == 0),
                             stop=(c == 15), r=[t, router_w], w=[pl])
        SC.pop()

    norm_phase(xT, hT, rstd, s1, sh1, "a")
    SC.push()
    wt_ = [S.sb(f"wq{i}", [128, 16, 256], BF16) for i in range(2)]
    sqb = [S.sb(f"qsq{i}", [128, 512], BF16) for i in range(2)]
    rsd = [S.sb(f"qrs{i}", [128, 512], F32) for i in range(2)]
    pk = [S.ps(f"pq{i}", [128, 512], F32) for i in range(3)]
    pn = [S.ps(f"pqn{i}", [128, 512], F32) for i in range(2)]
    n = 0
    for g2 in range(12):
        w_ = wt_[g2 % 2]
        wload(S, "pool", w_, w_[:, :, :], b_w_q, g2 * 256, 256)
        for c in range(2):
            ptile = g2 * 2 + c
            for half in range(2):
                hs = slice(half * 512, (half + 1) * 512)
                p_, q_, r_, n_ = pk[n % 3], sqb[n % 2], rsd[n % 2], pn[n % 2]
                n += 1
                for kc in range(16):
                    S.mm(p_[:, :], w_[:, kc, c * 128:(c + 1) * 128], hT[:, kc, hs], start=(kc == 0), stop=(kc == 15),
                         r=[w_, hT], w=[p_])
                S.act(q_[:, :], p_[:, :], AF.Square, r=[p_], w=[q_])
                S.mm(n_[:, :], bdones[:, :], q_[:, :], r=[q_], w=[n_])
                S.act(r_[:, :], n_[:, :], AF.Sqrt, scale=1.0 / 64, bias=epsc, r=[n_], w=[r_])
                S.v("dve", "reciprocal", r_[:, :], r_[:, :], r=[r_], w=[r_])
                S.v("dve", "scalar_tensor_tensor", qT1[:, ptile, hs], p_[:, :], gq[:, 0:1], r_[:, :], ALU.mult, ALU.mult,
                    r=[p_, r_], w=[qT1])
    SC.pop()
    SC.pop()
    if "qT1" in debug:
        d_ = dbg_out("qT1", [128, 24 * 1024], BF16)
        S.dma("sp", d_[:, :].rearrange("p (a b) -> p a b", a=24), qT1[:, :, :], r=[qT1], w=[d_])

    SC.push()
    relr = S.sb("relr", [32, 2304], F32)
    nn_ = S.sb("nn_", [32, 2304], F32)
    aa_ = S.sb("aa_", [32, 2304], F32)
    relbs = S.sb("relbs", [32, 16], F32)
    frow = S.sb("frow", [16, 2304], BF16)
    pfr = [S.ps(f"pfr{i}", [16, 512], F32) for i in range(2)]
    S.dma("sp", relr[:, :], relrow[:, :], r=[relrow], w=[relr])
    S.dma("sp", relbs[:, :], relb[:, :], r=[relb], w=[relbs])
    for kind in range(2):
        if kind == 0:
            S.v("dve", "tensor_scalar", nn_[:, :], relr[:, :], 0.0, None, ALU.max, r=[relr], w=[nn_])
        else:
            S.v("dve", "tensor_scalar", nn_[:, :], relr[:, :], cf[0:32, 264:265], 0.0, ALU.add, ALU.max,
                r=[relr], w=[nn_])
        S.v("dve", "tensor_scalar", aa_[:, :], nn_[:, :], cf[0:32, 265:266], None, ALU.is_ge, r=[nn_], w=[aa_])
        S.v("dve", "scalar_tensor_tensor", aa_[:, :], nn_[:, :], cf[0:32, 266:267], aa_[:, :], ALU.is_lt, ALU.mult,
            r=[nn_, aa_], w=[aa_])
        for pc in range(5):
            c0 = pc * 512
            pw = min(512, 2304 - c0)
            p_ = pfr[pc % 2]
            S.mm(p_[:, 0:pw], relbs[:, :], aa_[:, c0:c0 + pw], r=[relbs, aa_], w=[p_])
            S.act(frow[:, c0:c0 + pw], p_[:, 0:pw], AF.Copy, r=[p_], w=[frow])
        S.dma("sp", frowD[kind * 16:(kind + 1) * 16, :], frow[:, :], r=[frow], w=[frowD])
    SC.pop()

    SC.push()
    mk = S.sb("mk", [128, NT, 128], BF16)
    S.dma("pool", mk[:, :, :], maskl1[:, :].rearrange("p (a b) -> p a b", a=NT), r=[maskl1], w=[mk])
    Kt = [[S.sb(f"Kt{b}_{g}", [128, 2048], BF16) for g in range(3)] for b in range(2)]
    Vt = [[S.sb(f"Vt{b}_{g}", [128, 16, 128], BF16) for g in range(3)] for b in range(2)]
    Gp = [S.sb(f"Gp{b}", [128, 2, 16, 128], BF16) for b in range(2)]
    pT = [S.sb(f"pT{i}", [128, 4, 128], BF16) for i in range(3)]
    rden = [S.sb(f"rden{i}", [64, 128], F32) for i in range(2)]
    ostage = [S.sb(f"ostage{i}", [64, 2, 128], BF16) for i in range(3)]
    st = [S.ps(f"st{i}", [128, 4, 128], F32) for i in range(3)]
    po = [S.ps(f"po{i}", [128, 512], F32) for i in range(2)]
    pd = [S.ps(f"pd{i}", [128, 512], F32) for i in range(2)]
    KT_v = KT_all[:, :].rearrange("p (a b) -> p a b", a=24)
    oT_dv = oT_d[:, :].rearrange("p (h t) -> p h t", h=16)
    nst = 0
    nh = 0
    nos = 0
    for pr in range(8):
        b = pr % 2
        for g in range(3):
            S.dma("sp", Kt[b][g][:, :], KT_v[:, g * 8 + pr, :], r=[KT_all], w=[Kt[b][g]])
            S.dma("act", Vt[b][g][:, :, :],
                  V_all[:, g * 1024 + pr * 128:g * 1024 + (pr + 1) * 128].rearrange("(s p) d -> p s d", p=128),
                  r=[V_all], w=[Vt[b][g]])
        for hh in range(2):
            for kind in range(2):
                src = bass.AP(frowD.t.tensor, (kind * 16 + 2 * pr + hh) * 2304 + 129, [[1, 128], [256, 8], [1, 128]])
                S.dma("sp", Gp[b][:, hh, kind * 8:(kind + 1) * 8, :], src, r=[frowD], w=[Gp[b]])
        for j in range(8):
            og = ostage[nos % 3]
            nos += 1
            for hh in range(2):
                ps_ = slice(64 * hh, 64 * hh + 64)
                tiles = [(i, g, kind, t) for i, (g, kind, t) in enumerate(TL) if t <= j]
                nblk = len(tiles)
                po_, pd_, rd_ = po[nh % 2], pd[nh % 2], rden[nh % 2]
                nh += 1
                for g0 in range(0, nblk, 4):
                    grp = tiles[g0:g0 + 4]
                    st_, pT_ = st[nst % 3], pT[nst % 3]
                    nst += 1
                    for ii, (ti, g, kind, t) in enumerate(grp):
                        sb_ = kind * 8 + (j - t)
                        S.mm(st_[:, ii, :], Kt[b][g][ps_, sb_ * 128:(sb_ + 1) * 128],
                             qT1[ps_, g * 8 + pr, j * 128:(j + 1) * 128], start=True, stop=False,
                             r=[Kt[b][g]], w=[st_])
                        S.mm(st_[:, ii, :], antib[:, :], Gp[b][:, hh, kind * 8 + t, :], start=False, stop=False,
                             r=[Gp[b]], w=[st_])
                        S.mm(st_[:, ii, :], identb[:, :], mk[:, ti, :], start=False, stop=True, r=[mk], w=[st_])
                    ng = len(grp)
                    S.act(pT_[:, 0:ng, :], st_[:, 0:ng, :], AF.Exp, r=[st_], w=[pT_])
                    for ii, (ti, g, kind, t) in enumerate(grp):
                        i = g0 + ii
                        sb_ = kind * 8 + (j - t)
                        S.mm(po_[0:64, 0:128], Vt[b][g][:, sb_, hh * 64:(hh + 1) * 64], pT_[:, ii, :],
                             start=(i == 0), stop=(i == nblk - 1), r=[pT_, Vt[b][g]], w=[po_], sig=False)
                        S.mm(pd_[0:64, 0:128], onesb[:, 0:64], pT_[:, ii, :],
                             start=(i == 0), stop=(i == nblk - 1), r=[pT_], w=[pd_], sig=(ii == ng - 1))
                S.v("dve", "reciprocal", rd_[:, :], pd_[0:64, 0:128], r=[pd_], w=[rd_])
                S.v("dve", "tensor_tensor", og[:, hh, :], po_[0:64, 0:128], rd_[:, :], ALU.mult, r=[po_, pd_, rd_], w=[og])
            S.dma("sp", oT_dv[:, 2 * pr:2 * pr + 2, j * 128:(j + 1) * 128], og[:, :, :], r=[og], w=[oT_d])
    SC.pop()
    SC.pop()
    if stop_after == "att":
        SC.push()
        tmpo = S.sb("tmpo", [64, 16 * 1024], BF16)
        d_ = dbg_out("oT", [64, 16 * 1024], BF16)
        S.dma("sp", tmpo[:, :], oT_d[:, :], r=[oT_d], w=[tmpo])
        S.dma("sp", d_[:, :], tmpo[:, :], r=[tmpo], w=[d_])
        SC.pop()
        SC.pop()
        return nc, dbg
    SC.push()
    xT = S.sb("xT", [128, 16, 1024], F32)
    h2T = S.sb("h2T", [128, 16, 1024], BF16)
    rstd = S.sb("rstd", [128, 1024], F32)
    gbc = S.sb("gbc", [128, 8, 1024], BF16)
    S.dma("sp", xT[:, :, :], x1fm[:, :].rearrange("p (a b) -> p a b", a=16), r=[x1fm], w=[xT])
    SC.push()
    oT = S.sb("oT1", [64, 16, 1024], BF16)
    wbo = [S.sb(f"wbo{i}", [64, 16, 256], BF16) for i in range(2)]
    pa = [S.ps(f"pa{i}", [128, 512], F32) for i in range(3)]
    S.dma("sp", oT[:, :, :], oT_d[:, :].rearrange("p (h t) -> p h t", h=16), r=[oT_d], w=[oT])
    n = 0
    for fcg in range(8):
        w_ = wbo[fcg % 2]
        S.dma("pool", w_[:, :, :], b_w_out[:, fcg * 256:(fcg + 1) * 256].rearrange("(h d) n -> d h n", d=64),
              r=[b_w_out], w=[w_])
        for fc2 in range(2):
            fc = fcg * 2 + fc2
            for half in range(2):
                p_ = pa[n % 3]
                n += 1
                hs = slice(half * 512, (half + 1) * 512)
                for h in range(16):
                    S.mm(p_[:, :], w_[:, h, fc2 * 128:(fc2 + 1) * 128], oT[:, h, hs], start=(h == 0), stop=(h == 15),
                         r=[w_, oT], w=[p_])
                S.v("dve", "scalar_tensor_tensor", xT[:, fc, hs], p_[:, :], gt1[:, fc:fc + 1], xT[:, fc, hs],
                    ALU.mult, ALU.add, r=[p_, xT], w=[xT])
    SC.pop()
    if "xa" in debug:
        d_ = dbg_out("xa", [128, 16 * 1024])
        S.dma("sp", d_[:, :].rearrange("p (a b) -> p a b", a=16), xT[:, :, :], r=[xT], w=[d_])

    SC.push()
    rw = S.sb("rw", [128, 16, 8], F32)
    rbs = S.sb("rbs", [128, 8], F32)
    sels = S.sb("sels", [8, 8 * 128], F32)
    lg = S.sb("lg", [128, 8, 8], F32)
    dg = S.sb("dg", [128, 8, 8], F32)
    m8 = S.sb("m8", [128, 8, 8], F32)
    e2 = S.sb("e2", [128, 8, 4], F32)
    eq = S.sb("eq", [128, 8, 8], F32)
    dgT = S.sb("dgT", [8, 1024], F32)
    plog = [S.ps(f"plog{i}", [128, 512], F32) for i in range(4)]
    S.dma("sp", rw[:, :, :], router[:, :].rearrange("(c p) e -> p c e", p=128), r=[router], w=[rw])
    S.dma("sp", rbs[:, :], rb_bc[:, :], r=[rb_bc], w=[rbs])
    S.dma("sp", sels[:, :], selc[:, :], r=[selc], w=[sels])
    plog8 = [plog[i % 4] for i in range(8)]

    def norm_router():
        SC.push()
        sq = [S.sb(f"sq{i}_r", [128, 512], BF16) for i in range(2)]
        tmp = [S.sb(f"tmpn{i}_r", [128, 512], F32) for i in range(2)]
        pss = [S.ps(f"pss{i}_r", [128, 512], F32) for i in range(2)]
        for half in range(2):
            cs_ = slice(half * 512, (half + 1) * 512)
            ps_ = pss[half]
            for c in range(16):
                q = sq[c % 2]
                S.act(q[:, :], xT[:, c, cs_], AF.Square, r=[xT], w=[q])
                S.mm(ps_[:, :], onesb[:, :], q[:, :], start=(c == 0), stop=(c == 15), r=[q], w=[ps_])
            S.act(rstd[:, cs_], ps_[:, :], AF.Sqrt, scale=1.0 / 2048, bias=epsc, r=[ps_], w=[rstd])
            S.v("dve", "reciprocal", rstd[:, cs_], rstd[:, cs_], r=[rstd], w=[rstd])
            for c in range(16):
                t = tmp[c % 2]
                S.v("dve", "scalar_tensor_tensor", t[:, :], xT[:, c, cs_], s2[:, c:c + 1], rstd[:, cs_],
                    ALU.mult, ALU.mult, r=[xT, rstd], w=[t])
                S.v("dve", "tensor_scalar", t[:, :], t[:, :], sh2[:, c:c + 1], None, ALU.add, r=[t], w=[t])
                S.act(h2T[:, c, cs_], t[:, :], AF.Copy, r=[t], w=[h2T])
                for b4 in range(4):
                    pl = plog[b4]
                    S.mm(pl[:, 0:8], t[:, b4 * 128:(b4 + 1) * 128], rw[:, c, :], start=(c == 0),
                         stop=(c == 15), r=[t, rw], w=[pl])
            for b4 in range(4):
                tb = half * 4 + b4
                S.v("dve", "tensor_tensor", lg[:, tb, :], plog[b4][:, 0:8], rbs[:, :], ALU.add,
                    r=[plog[b4], rbs], w=[lg])
        SC.pop()

    norm_router()
    if "lg" in debug:
        d_ = dbg_out("lg", [128, 64])
        S.dma("sp", d_[:, :].rearrange("p (a b) -> p a b", a=8), lg[:, :, :], r=[lg], w=[d_])
    for tb in range(8):
        S.v("dve", "max", m8[:, tb, :], lg[:, tb, :], r=[lg], w=[m8])
        S.v("dve", "tensor_tensor", e2[:, tb, 0:1], m8[:, tb, 1:2], m8[:, tb, 0:1], ALU.subtract, r=[m8], w=[e2])
        S.act(e2[:, tb, 0:1], e2[:, tb, 0:1], AF.Exp, r=[e2], w=[e2])
        S.v("dve", "tensor_scalar", e2[:, tb, 1:2], e2[:, tb, 0:1], 1.0, None, ALU.add, r=[e2], w=[e2])
        S.v("dve", "reciprocal", e2[:, tb, 1:2], e2[:, tb, 1:2], r=[e2], w=[e2])
        S.v("dve", "tensor_tensor", e2[:, tb, 2:3], e2[:, tb, 0:1], e2[:, tb, 1:2], ALU.mult, r=[e2], w=[e2])
        S.v("dve", "tensor_scalar", dg[:, tb, :], lg[:, tb, :], m8[:, tb, 0:1], e2[:, tb, 1:2], ALU.is_equal, ALU.mult,
            r=[lg, m8, e2], w=[dg])
        S.v("dve", "tensor_scalar", eq[:, tb, :], lg[:, tb, :], m8[:, tb, 1:2], e2[:, tb, 2:3], ALU.is_equal, ALU.mult,
            r=[lg, m8, e2], w=[eq])
        S.v("dve", "tensor_tensor", dg[:, tb, :], dg[:, tb, :], eq[:, tb, :], ALU.add, r=[dg, eq], w=[dg])
    for tb in range(8):
        pl = plog[tb % 4]
        S.tr(pl[0:8, 0:128], dg[:, tb, :], cf[:, 0:128], r=[dg], w=[pl])
        S.v("dve", "tensor_copy", dgT[:, tb * 128:(tb + 1) * 128], pl[0:8, 0:128], r=[pl], w=[dgT])
    for e in range(8):
        for half in range(2):
            pl = plog[(e * 2 + half) % 4]
            hs = slice(half * 512, (half + 1) * 512)
            S.mm(pl[:, :], sels[:, e * 128:(e + 1) * 128], dgT[:, hs], r=[sels, dgT], w=[pl])
            S.act(gbc[:, e, hs], pl[:, :], AF.Copy, r=[pl], w=[gbc])
    if "dg" in debug:
        d_ = dbg_out("dg", [128, 64])
        S.dma("sp", d_[:, :].rearrange("p (a b) -> p a b", a=8), dg[:, :, :], r=[dg], w=[d_])
    SC.pop()
    if stop_after == "router":
        SC.pop(); SC.pop()
        return nc, dbg

    SC.push()
    w1g = [S.sb(f"w1g{i}", [128, 16, 256], BF16) for i in range(2)]
    w3g = [S.sb(f"w3g{i}", [128, 16, 256], BF16) for i in range(2)]
    w2g = [S.sb(f"w2g{i}", [128, 2, 2048], BF16) for i in range(2)]
    ug = [S.sb(f"ug{i}", [128, 2, 1024], BF16) for i in range(2)]
    sil = [S.sb(f"sil{i}", [128, 512], F32) for i in range(2)]
    sil2 = [S.sb(f"silb{i}", [128, 512], F32) for i in range(2)]
    p1 = [S.ps(f"p1{i}", [128, 512], F32) for i in range(2)]
    p3 = [S.ps(f"p3{i}", [128, 512], F32) for i in range(2)]
    py = [S.ps(f"py{i}", [128, 512], F32) for i in range(3)]
    tmpy = [S.sb(f"tmpy{i}", [128, 512], F32) for i in range(3)]
    n13 = 0
    ny = 0
    gi = 0
    for e in range(8):
        for g in range(28):
            a1, a3, a2, u_ = w1g[gi % 2], w3g[gi % 2], w2g[gi % 2], ug[gi % 2]
            gi += 1
            wload(S, "pool", a1, a1[:, :, :], moe_w1, g * 256, 256, r0=e * 2048)
            wload(S, "pool", a3, a3[:, :, :], moe_w3, g * 256, 256, r0=e * 2048)
            for c in range(2):
                for hh in range(2):
                    r0 = e * 7168 + g * 256 + c * 128
                    S.dma("pool", a2[:, c, hh * 1024:(hh + 1) * 1024],
                          moe_w2[r0:r0 + 128, hh * 1024:(hh + 1) * 1024], r=[moe_w2], w=[a2])
            for c in range(2):
                for half in range(2):
                    hs = slice(half * 512, (half + 1) * 512)
                    q1, q3, sl, sl2 = p1[n13 % 2], p3[n13 % 2], sil[n13 % 2], sil2[n13 % 2]
                    n13 += 1
                    for kc in range(16):
                        S.mm(q1[:, :], a1[:, kc, c * 128:(c + 1) * 128], h2T[:, kc, hs], start=(kc == 0),
                             stop=(kc == 15), r=[a1, h2T], w=[q1])
                    for kc in range(16):
                        S.mm(q3[:, :], a3[:, kc, c * 128:(c + 1) * 128], h2T[:, kc, hs], start=(kc == 0),
                             stop=(kc == 15), r=[a3, h2T], w=[q3])
                    S.act(sl[:, :], q1[:, :], AF.Silu, r=[q1], w=[sl])
                    S.v("pool", "tensor_tensor", sl2[:, :], sl[:, :], gbc[:, e, hs], ALU.mult, r=[sl, gbc], w=[sl2])
                    S.v("dve", "tensor_tensor", u_[:, c, hs], sl2[:, :], q3[:, :], ALU.mult, r=[sl2, q3], w=[u_])
            for fc in range(16):
                for half in range(2):
                    hs = slice(half * 512, (half + 1) * 512)
                    y_ = py[ny % 3]
                    ny += 1
                    for c in range(2):
                        S.mm(y_[:, :], a2[:, c, fc * 128:(fc + 1) * 128], u_[:, c, hs], start=(c == 0), stop=(c == 1),
                             r=[a2, u_], w=[y_])
                    xk = xT.k((fc, half))
                    if fc % 2 == 0:
                        S.v("dve", "scalar_tensor_tensor", xT[:, fc, hs], y_[:, :], gt2[:, fc:fc + 1], xT[:, fc, hs],
                            ALU.mult, ALU.add, r=[y_, xk], w=[xk])
                    else:
                        ty = tmpy[ny % 3]
                        S.act(ty[:, :], y_[:, :], AF.Identity, scale=gt2[:, fc:fc + 1], bias=0.0, r=[y_], w=[ty])
                        S.v("pool", "tensor_tensor", xT[:, fc, hs], xT[:, fc, hs], ty[:, :], ALU.add,
                            r=[ty, xk], w=[xk])
    SC.pop()
    S.dma("sp", out_fm[:, :].rearrange("p (a b) -> p a b", a=16), xT[:, :, :],
          r=[xT] + [xT.k((fc, half)) for fc in range(16) for half in range(2)], w=[out_fm])
    SC.pop()
    SC.pop()
    return nc, dbg


def prep_B(inp, resA):
    TL = l1_tiles()
    selc = np.zeros((8, 8 * 128), np.float32)
    for e in range(8):
        selc[e, e * 128:(e + 1) * 128] = 1.0
    maps = []
    for core in range(8):
        b, par = core // 2, core % 2
        own, prt = resA[core], resA[core ^ 1]
        kt = np.concatenate([np.asarray(own["kT_out"]).reshape(128, 24, 1024),
                             np.asarray(prt["kT_out"]).reshape(128, 24, 1024)], axis=2).reshape(128, 24 * 2048)
        vv = np.concatenate([np.asarray(own["v_out"]), np.asarray(prt["v_out"])], axis=0)
        gsm = np.zeros((128, 4), np.float32)
        gsm[:, 0] = np.tile(inp["b_g_qn"][0], 2)
        m = {
            "x1fm": np.asarray(own["x1_out"]),
            "KT_all": np.ascontiguousarray(kt),
            "V_all": np.ascontiguousarray(vv),
            "c_fm": fm16(inp["c"][b]),
            "consts": make_consts(par),
            "relrow": make_relrow(),
            "rel_bias": np.ascontiguousarray(inp["rel_bias"]),
            "gvb": np.concatenate([fm16(inp["g_attn"][1]), fm16(inp["g_ffn"][1])], axis=1),
            "gsmb": gsm,
            "maskl1": make_l1_masks(par),
            "selc": selc,
            "rb_bc": np.ascontiguousarray(np.broadcast_to(inp["moe_router_b"][0][None, :], (128, 8))),
            "w_mod1": inp["w_mod"][1], "b_mod1": inp["b_mod"][1][None, :],
            "b_w_q": inp["b_w_q"][0], "b_w_out": inp["b_w_out"][0],
            "router": inp["moe_router"][0],
            "moe_w1": inp["moe_w1"][0].reshape(8 * 2048, 7168),
            "moe_w3": inp["moe_w3"][0].reshape(8 * 2048, 7168),
            "moe_w2": inp["moe_w2"][0].reshape(8 * 7168, 2048),
        }
        maps.append(m)
    return maps


def assemble_out(resB):
    out = np.zeros((4, 2048, 2048), np.float32)
    for core in range(8):
        b, par = core // 2, core % 2
        o = np.asarray(resB[core]["out_fm"]).reshape(128, 16, 1024).transpose(2, 1, 0).reshape(8, 128, 2048)
        for j in range(8):
            blk = 2 * j + par
            out[b, blk * 128:(blk + 1) * 128] = o[j]
    return out


_CACHE = {}


def kernel(**inputs):
    inp = {k: np.asarray(v) for k, v in inputs.items()}
    if "F" not in _CACHE:
        _CACHE["F"] = build_fused()
    rb = run_bass_kernel_spmd(_CACHE["F"], prep_fused(inp), core_ids=list(range(8)))
    return assemble_out(rb.results)


def build_fused():
    nc = bass.Bass("TRN2", target_bir_lowering=False)
    S = Sched(nc)
    SC = Scope(S)
    ctx = {"nc": nc, "S": S, "SC": SC}
    build_A(ctx=ctx)
    S.prefix = "zx_"
    kT_out, v_out, KT_all, V_all = ctx["kT_out"], ctx["v_out"], ctx["KT_all"], ctx["V_all"]
    consts = S.ext_cache["consts"]
    Gk = S.dram("Gk", [8 * 128, 24 * 1024], BF16)
    Gv = S.dram("Gv", [8 * 1024, 3072], BF16)
    SC.push()
    groups = [list(range(8))]
    ccs = [S.es.enter_context(nc.semaphore(f"cc{i}")) for i in range(2)]

    def cc(i, src, dst):
        def f(e):
            ins = e.collective_compute("AllGather", ALU.bypass, replica_groups=groups,
                                       ins=[src.t.opt()], outs=[dst.t.opt()])
            ins.then_inc(ccs[i], CC_INC)
            e.wait_ge(ccs[i], CC_INC)
            return ins
        return f

    S.flush()
    S.op("pool", cc(0, kT_out, Gk), r=[kT_out], w=[], sig=False)
    S.op("pool", cc(1, v_out, Gv), r=[v_out], w=[], sig=False)
    KT_allv = KT_all[:, :].rearrange("p (a b) -> p a b", a=24)
    Gkv = Gk[:, :].rearrange("p (a b) -> p a b", a=24)

    def cpk(e):
        pid = e.partition_id()
        partner = pid + 1 - 2 * (pid % 2)
        return e.dma_start(out=KT_allv[:, :, 1024:2048], in_=Gkv[bass.ds(partner * 128, 128), :, :])

    def cpv(e):
        pid = e.partition_id()
        partner = pid + 1 - 2 * (pid % 2)
        return e.dma_start(out=V_all[1024:2048, :], in_=Gv[bass.ds(partner * 1024, 1024), :])

    S.op("pool", cpk, w=[KT_all], dma=True)
    S.op("pool", cpv, w=[V_all], dma=True)
    SC.pop()
    build_B(ctx=ctx)
    return nc


CC_INC = 1


def prep_fused(inp):
    A = prep_A(inp)
    TL = l1_tiles()
    selc = np.zeros((8, 8 * 128), np.float32)
    for e in range(8):
        selc[e, e * 128:(e + 1) * 128] = 1.0
    maps = []
    for core in range(8):
        b, par = core // 2, core % 2
        gsm = np.zeros((128, 4), np.float32)
        gsm[:, 0] = np.tile(inp["b_g_qn"][0], 2)
        m = dict(A[core])
        m.update({
            "gvb": np.concatenate([fm16(inp["g_attn"][1]), fm16(inp["g_ffn"][1])], axis=1),
            "gsmb": gsm,
            "maskl1": make_l1_masks(par),
            "selc": selc,
            "rb_bc": np.ascontiguousarray(np.broadcast_to(inp["moe_router_b"][0][None, :], (128, 8))),
            "w_mod1": inp["w_mod"][1], "b_mod1": inp["b_mod"][1][None, :],
            "b_w_q": inp["b_w_q"][0], "b_w_out": inp["b_w_out"][0],
            "router": inp["moe_router"][0],
            "moe_w1": inp["moe_w1"][0].reshape(8 * 2048, 7168),
            "moe_w3": inp["moe_w3"][0].reshape(8 * 2048, 7168),
            "moe_w2": inp["moe_w2"][0].reshape(8 * 7168, 2048),
        })
        maps.append(m)
    return maps
```

```python
import math
from contextlib import ExitStack

import numpy as np
import ml_dtypes
import concourse.bass as bass
import concourse.mybir as mybir
from concourse.bass_utils import run_bass_kernel_spmd

F32 = mybir.dt.float32
BF16 = mybir.dt.bfloat16
I32 = mybir.dt.int32
AF = mybir.ActivationFunctionType
ALU = mybir.AluOpType
AX = mybir.AxisListType

D = 2048
SEQ = 2048
NB = 16
OWN = 8
TOWN = 1024
EPS = 1e-6
NEG = -30000.0
BIGNEG = -1.0e30


class Tk:
    __slots__ = ("name", "w", "rs")

    def __init__(self, name):
        self.name = name
        self.w = None
        self.rs = []


class Buf:
    def __init__(self, t, name, n=1):
        self.t = t
        self.name = name
        self.tk = Tk(name)
        self.sub = {}

    def __getitem__(self, k):
        return self.t[k]

    def k(self, key):
        if key not in self.sub:
            self.sub[key] = Tk(f"{self.name}.{key}")
        return self.sub[key]


def _tk(x):
    return x.tk if isinstance(x, Buf) else x


class Op:
    __slots__ = ("eng", "fn", "deps", "dma", "sig", "tok", "cnt")


NDSEM = 12


class Sched:
    ENGS = ("pe", "act", "dve", "pool", "sp")

    def __init__(self, nc):
        self.nc = nc
        self.es = ExitStack()
        self.sem = {e: self.es.enter_context(nc.semaphore(f"s_{e}")) for e in self.ENGS}
        self.dsem = {q: [self.es.enter_context(nc.semaphore(f"d_{q}{i}")) for i in range(NDSEM)]
                     for q in ("sp", "act", "pool")}
        self.cnt = {e: 0 for e in self.ENGS}
        self.ndma = {q: 0 for q in ("sp", "act", "pool")}
        self.ops = {e: [] for e in self.ENGS}
        self.trackers = []
        self.pending = {e: set() for e in self.ENGS}
        self.seen = {e: {} for e in self.ENGS}
        self.phase_es = None
        self.nops = 0
        self.prefix = ""
        self.ext_cache = {}

    def begin(self):
        self.phase_es = ExitStack()

    def sb(self, name, shape, dtype):
        name = self.prefix + name
        t = self.phase_es.enter_context(self.nc.sbuf_tensor(name, list(shape), dtype))
        return Buf(t, name)

    def ps(self, name, shape, dtype=F32):
        name = self.prefix + name
        t = self.phase_es.enter_context(self.nc.psum_tensor(name, list(shape), dtype))
        return Buf(t, name)

    def dram(self, name, shape, dtype, kind=None):
        if kind == "ExternalInput":
            if name in self.ext_cache:
                return self.ext_cache[name]
        if kind is None:
            name = self.prefix + name
            t = self.nc.dram_tensor(name, list(shape), dtype)
        else:
            t = self.nc.dram_tensor(name, list(shape), dtype, kind=kind)
        b = Buf(t.ap(), name)
        if kind == "ExternalInput":
            self.ext_cache[name] = b
        return b

    def op(self, eng, fn, r=(), w=(), dma=False, sig=True):
        o = Op()
        o.eng = eng
        o.fn = fn
        o.dma = dma
        o.sig = sig
        deps = set(self.pending[eng])
        self.pending[eng] = set()
        rt = [_tk(x) for x in r]
        wt = [_tk(x) for x in w]
        for t in rt:
            if t.w is not None:
                deps.add(t.w)
        for t in wt:
            if t.w is not None:
                deps.add(t.w)
            for x in t.rs:
                deps.add(x)
        idx = len(self.ops[eng])
        if dma:
            n = self.ndma[eng]
            self.ndma[eng] += 1
            tok = ("d", eng, n)
            if n >= NDSEM:
                deps.add(("d", eng, n - NDSEM))
        else:
            tok = ("e", eng, idx)
        o.tok = tok
        best = {}
        out = []
        for d in deps:
            if d[0] == "e":
                if d[1] == "pe" and eng == "pe" and not dma:
                    continue
                if d[1] not in best or best[d[1]] < d[2]:
                    best[d[1]] = d[2]
            else:
                out.append(d)
        for e_, i_ in best.items():
            out.append(("e", e_, i_))
            self.ops[e_][i_].sig = True
        o.deps = out
        for t in rt:
            t.rs = [x for x in t.rs if not (x[0] == "e" and tok[0] == "e" and x[1] == tok[1])] + [tok]
            if t not in self.trackers:
                self.trackers.append(t)
        for t in wt:
            t.w = tok
            t.rs = []
            if t not in self.trackers:
                self.trackers.append(t)
        self.ops[eng].append(o)
        self.nops += 1
        return o

    def mm(self, out, lhsT, rhs, start=True, stop=True, r=(), w=(), sig=None):
        if sig is None:
            sig = stop
        return self.op("pe", lambda e: e.matmul(out, lhsT, rhs, start=start, stop=stop), r, w, sig=sig)

    def tr(self, out, in_, ident, r=(), w=()):
        return self.op("pe", lambda e: e.transpose(out, in_, ident), r, w)

    def act(self, out, in_, func, r=(), w=(), **kw):
        return self.op("act", lambda e: e.activation(out, in_, func, **kw), r, w)

    def v(self, eng, name, *a, r=(), w=(), **kw):
        return self.op(eng, lambda e: getattr(e, name)(*a, **kw), r, w)

    def dma(self, q, out, in_, r=(), w=(), **kw):
        return self.op(q, lambda e: e.dma_start(out=out, in_=in_, **kw), r, w, dma=True)

    def nops_pending(self):
        return sum(len(v) for v in self.ops.values())

    def _engine(self, block, e):
        return {"pe": block.tensor, "act": block.scalar, "dve": block.vector,
                "pool": block.gpsimd, "sp": block.sync}[e]

    def end(self, final=False):
        self.flush()
        self.phase_es.close()
        self.phase_es = None

    def flush(self, final=False):
        if self.nops_pending() == 0:
            return
        pe_ops = self.ops["pe"]
        if pe_ops:
            pe_ops[-1].sig = True
        for e in self.ENGS:
            if self.ops[e] and not self.ops[e][-1].dma:
                self.ops[e][-1].sig = True
        nxt = [None] * len(pe_ops)
        last = None
        for i in range(len(pe_ops) - 1, -1, -1):
            if pe_ops[i].sig:
                last = i
            nxt[i] = last
        cntmap = {}
        for e in self.ENGS:
            c = self.cnt[e]
            for i, o in enumerate(self.ops[e]):
                if o.dma:
                    continue
                if o.sig:
                    c += 1
                    o.cnt = c
                    cntmap[(e, i)] = c
            self.cnt[e] = c

        def resolve(tok):
            if tok[0] == "d":
                _, q, n = tok
                return self.dsem[q][n % NDSEM], 16 * (n // NDSEM + 1)
            _, e, i = tok
            if e == "pe" and (e, i) not in cntmap:
                i = nxt[i]
            return self.sem[e], cntmap[(e, i)]

        base_idx = dict(self.phase_base) if hasattr(self, "phase_base") else {}

        def emit(e, eng):
            seen = self.seen[e]
            for o in self.ops[e]:
                for d in o.deps:
                    if d[0] == "e" and d[2] < 0:
                        continue
                    sem, val = resolve(d)
                    key = sem.num if hasattr(sem, "num") else id(sem)
                    if seen.get(key, 0) >= val:
                        continue
                    seen[key] = val
                    eng.wait_ge(sem, val)
                ins = o.fn(eng)
                if o.dma:
                    _, q, n = o.tok
                    ins.then_inc(self.dsem[q][n % NDSEM], 16)
                elif o.sig:
                    ins.then_inc(self.sem[e], 1)

        with self.nc.Block() as block:
            for e in self.ENGS:
                if not self.ops[e]:
                    continue

                def f(eng, e=e):
                    emit(e, eng)
                self._engine(block, e)(f)

        allt = set()
        for e in self.ENGS:
            if self.cnt[e] > 0:
                allt.add(("c", e, self.cnt[e]))
        for q in ("sp", "act", "pool"):
            n = self.ndma[q]
            for k in range(max(0, n - NDSEM), n):
                allt.add(("d", q, k))
        self.barrier_tokens = allt
        for e in self.ENGS:
            self.ops[e] = []
        for t in self.trackers:
            t.w = None
            t.rs = []
        self.trackers = []
        self._emit_barrier(final)

    def _emit_barrier(self, final):
        with self.nc.Block() as block:
            for e in self.ENGS:
                def f(eng, e=e):
                    seen = self.seen[e]
                    for t in self.barrier_tokens:
                        if t[0] == "c":
                            sem, val = self.sem[t[1]], t[2]
                        else:
                            _, q, n = t
                            sem, val = self.dsem[q][n % NDSEM], 16 * (n // NDSEM + 1)
                        key = sem.num if hasattr(sem, "num") else id(sem)
                        if seen.get(key, 0) >= val:
                            continue
                        seen[key] = val
                        eng.wait_ge(sem, val)
                self._engine(block, e)(f)


class Scope:
    def __init__(self, S):
        self.S = S
        self.stack = []

    def push(self):
        es = ExitStack()
        self.stack.append(es)
        self.S.phase_es = es

    def pop(self):
        self.S.flush()
        self.stack.pop().close()
        self.S.phase_es = self.stack[-1] if self.stack else None


def t5_thresholds():
    n = np.arange(0, 8192, dtype=np.int64)
    nf = np.maximum(n, 1).astype(np.float32)
    large = 16 + (np.log(nf / np.float32(16)) / np.float32(math.log(2048 / 16))
                  * np.float32(16)).astype(np.int32)
    large = np.minimum(large, 31)
    b = np.where(n < 16, n, large)
    T = [int(np.argmax(b >= k)) for k in range(32)]
    return T


NCONST = 400


def make_consts(parity):
    c = np.zeros((128, NCONST), np.float32)
    c[:, 0:128] = np.eye(128, dtype=np.float32)
    c[0:64, 128:192] = 1.0
    c[64:128, 192:256] = 1.0
    p = np.arange(128)
    for j in range(8):
        c[:, 256 + j] = (2 * j + parity) * 128 + p
    c[:, 264] = 128.0 * (2 * parity - 1)
    T = t5_thresholds()
    for b in range(32):
        c[b, 265] = T[b]
        c[b, 266] = T[b + 1] if b < 31 else 1e9
    c[:, 267] = p
    c[:, 268] = 1.0
    c[:, 269] = EPS
    c[:, 270] = float(parity)
    c[:, 271] = float(1 - parity)
    c[:, 272:400] = np.eye(128, dtype=np.float32)[::-1]
    return c


def make_relrow():
    return np.ascontiguousarray(np.broadcast_to((np.arange(2304, dtype=np.float32) - 256.0)[None, :], (32, 2304)))


def make_kidx(parity):
    k = np.zeros((1, 2048), np.float32)
    for m in range(8):
        k[0, m * 128:(m + 1) * 128] = (2 * m + parity) * 128 + np.arange(128)
        k[0, 1024 + m * 128:1024 + (m + 1) * 128] = (2 * m + 1 - parity) * 128 + np.arange(128)
    return k


def storage_order(parity):
    return [2 * m + parity for m in range(8)] + [2 * m + 1 - parity for m in range(8)]


WQ = ("sp", "act")


def mod_rows(S, SC, cs, W, Bv, N, out_fm, ident1, name):
    SC.push()
    wt = [S.sb(f"{name}_wt{i}", [128, 1024], F32) for i in range(4)]
    row = S.sb(f"{name}_row", [1, N], F32)
    brow = S.sb(f"{name}_brow", [1, N], F32)
    prow = [S.ps(f"{name}_pr{i}", [1, 1024], F32) for i in range(2)]
    pf = S.ps(f"{name}_pf", [128, 128], F32)
    S.dma("sp", brow[:, :], Bv[:, :], r=[Bv], w=[brow])
    n = 0
    for nb in range(N // 1024):
        pr = prow[nb % 2]
        for k in range(16):
            t = wt[n % 4]
            S.dma(WQ[n % 2], t[:, :], W[k * 128:(k + 1) * 128, nb * 1024:(nb + 1) * 1024], r=[W], w=[t])
            n += 1
            for h in range(2):
                S.mm(pr[0:1, h * 512:(h + 1) * 512], cs[:, k:k + 1], t[:, h * 512:(h + 1) * 512],
                     start=(k == 0), stop=(k == 15), r=[cs, t], w=[pr], sig=(k == 15 and h == 1) or True)
        S.v("dve", "tensor_tensor", row[0:1, nb * 1024:(nb + 1) * 1024], pr[0:1, :],
            brow[0:1, nb * 1024:(nb + 1) * 1024], ALU.add, r=[pr, brow], w=[row])
    nj = N // 128
    for j in range(nj):
        S.mm(pf[:, j:j + 1], row[0:1, j * 128:(j + 1) * 128], ident1[0:1, 0:1], start=True, stop=True,
             r=[row, ident1], w=[pf])
    S.v("dve", "tensor_copy", out_fm[:, 0:nj], pf[:, 0:nj], r=[pf], w=[out_fm])
    SC.pop()


def rms_fm(S, xT, ncols, onesb, out_rstd, sq, pss, epsc):
    for half in range(ncols // 512):
        cs_ = slice(half * 512, (half + 1) * 512)
        ps_ = pss[half % len(pss)]
        for c in range(16):
            q = sq[c % len(sq)]
            S.act(q[:, :], xT[:, c, cs_], AF.Square, r=[xT], w=[q])
            S.mm(ps_[:, :], onesb[:, :], q[:, :], start=(c == 0), stop=(c == 15), r=[onesb, q], w=[ps_])
        S.act(out_rstd[:, cs_], ps_[:, :], AF.Sqrt, scale=1.0 / 2048, bias=epsc, r=[ps_], w=[out_rstd])
        S.v("dve", "reciprocal", out_rstd[:, cs_], out_rstd[:, cs_], r=[out_rstd], w=[out_rstd])


def norm_mod_fm(S, xT, ncols, rstd, scol, bcol, outT, tmp, name):
    n = 0
    for half in range(ncols // 512):
        cs_ = slice(half * 512, (half + 1) * 512)
        for c in range(16):
            t = tmp[n % len(tmp)]
            n += 1
            S.v("dve", "scalar_tensor_tensor", t[:, :], xT[:, c, cs_], scol[:, c:c + 1], rstd[:, cs_],
                ALU.mult, ALU.mult, r=[xT, scol, rstd], w=[t])
            S.act(outT[:, c, cs_], t[:, :], AF.Identity, bias=bcol[:, c:c + 1], scale=1.0,
                  r=[t, bcol], w=[outT])


def wload(S, q, dst, dst_ap, W, c0, n, r0=0, nk=16):
    src = W[r0:r0 + nk * 128, c0:c0 + n].rearrange("(k p) n -> p k n", p=128)
    S.dma(q, dst_ap, src, r=[W], w=[dst])


def build_A(stop_after=None, debug=(), ctx=None):
    if ctx is None:
        nc = bass.Bass("TRN2", target_bir_lowering=False)
        S = Sched(nc)
        SC = Scope(S)
    else:
        nc, S, SC = ctx["nc"], ctx["S"], ctx["SC"]
    S.prefix = "za_"
    dbg = {}

    def ext(name, shape, dt=F32):
        return S.dram(name, shape, dt, kind="ExternalInput")

    x_all = ext("x_all", [2048, 2048])
    c_fm = ext("c_fm", [128, 16])
    consts = ext("consts", [128, NCONST])
    kidx = ext("kidx", [128, 2048])
    relrow = ext("relrow", [32, 2304])
    relb = ext("rel_bias", [32, 16])
    gv = ext("gv", [128, 3 * 16])
    gsm = ext("gsm", [128, 4])
    w_mod0 = ext("w_mod0", [2048, 12288])
    b_mod0 = ext("b_mod0", [1, 12288])
    kv_w_mod = ext("kv_w_mod", [2048, 4096])
    kv_b_mod = ext("kv_b_mod", [1, 4096])
    a_w_in = ext("a_w_in", [2048, 4176])
    a_w_out = ext("a_w_out", [2048, 2048])
    ffn_w1 = ext("ffn_w1", [2048, 5632])
    ffn_w3 = ext("ffn_w3", [2048, 5632])
    ffn_w2 = ext("ffn_w2", [5632, 2048])
    kv_w = ext("kv_w", [2048, 6144])

    okind = "ExternalOutput" if ctx is None else None
    x1_out = S.dram("x1_out", [128, 16 * 1024], F32, kind=okind)
    kT_out = S.dram("kT_out", [128, 24 * 1024], BF16, kind=okind)
    v_out = S.dram("v_out", [1024, 3072], BF16, kind=okind)
    if ctx is not None:
        ctx["x1_out"], ctx["kT_out"], ctx["v_out"] = x1_out, kT_out, v_out
        KT_all = S.dram("KT_all", [128, 24 * 2048], BF16)
        V_all = S.dram("V_all", [2048, 3072], BF16)
        ctx["KT_all"], ctx["V_all"] = KT_all, V_all
        KT_allv = KT_all[:, :].rearrange("p (a b) -> p a b", a=24)
    frowD = S.dram("frowD", [2 * 16, 2304], BF16)
    oT_d = S.dram("oT_d", [128, 16 * 1024], BF16)

    def dbg_out(name, shape, dt=F32):
        b = S.dram("dbg_" + name, shape, dt, kind="ExternalOutput")
        dbg[name] = b
        return b

    SC.push()
    cf = S.sb("cf", [128, NCONST], F32)
    identb = S.sb("identb", [128, 128], BF16)
    antib = S.sb("antib", [128, 128], BF16)
    identf = cf
    bdones = S.sb("bdones", [128, 128], BF16)
    onesb = S.sb("onesb", [128, 128], BF16)
    cs = S.sb("cs", [128, 16], F32)
    mod0 = S.sb("mod0", [128, 96], F32)
    kvm = S.sb("kvm", [128, 32], F32)
    gvs = S.sb("gvs", [128, 48], F32)
    gs = S.sb("gs", [128, 4], F32)
    s1 = S.sb("s1", [128, 16], F32)
    s2 = S.sb("s2", [128, 16], F32)
    skv = S.sb("skv", [128, 16], F32)
    gq = S.sb("gq", [128, 1], F32)
    S.dma("sp", cf[:, :], consts[:, :], r=[consts], w=[cf])
    S.dma("pool", identb[:, :], consts[:, 0:128], r=[consts], w=[identb])
    S.dma("pool", bdones[:, :], consts[:, 128:256], r=[consts], w=[bdones])
    S.dma("pool", antib[:, :], consts[:, 272:400], r=[consts], w=[antib])
    S.dma("sp", cs[:, :], c_fm[:, :], r=[c_fm], w=[cs])
    S.dma("sp", gvs[:, :], gv[:, :], r=[gv], w=[gvs])
    S.dma("sp", gs[:, :], gsm[:, :], r=[gsm], w=[gs])
    S.v("dve", "memset", onesb[:, :], 1.0, w=[onesb])
    S.act(cs[:, :], cs[:, :], AF.Silu, r=[cs], w=[cs])
    mod_rows(S, SC, cs, w_mod0, b_mod0, 12288, mod0, cf, "m0")
    mod_rows(S, SC, cs, kv_w_mod, kv_b_mod, 4096, kvm, cf, "mk")
    S.v("dve", "scalar_tensor_tensor", s1[:, :], mod0[:, 16:32], 1.0, gvs[:, 0:16], ALU.add, ALU.mult,
        r=[mod0, gvs], w=[s1])
    S.v("dve", "scalar_tensor_tensor", s2[:, :], mod0[:, 64:80], 1.0, gvs[:, 16:32], ALU.add, ALU.mult,
        r=[mod0, gvs], w=[s2])
    S.v("dve", "scalar_tensor_tensor", skv[:, :], kvm[:, 16:32], 1.0, gvs[:, 32:48], ALU.add, ALU.mult,
        r=[kvm, gvs], w=[skv])
    S.v("dve", "tensor_scalar", gq[:, :], gs[:, 0:1], 128.0 ** -0.5, None, ALU.mult, r=[gs], w=[gq])
    epsc = cf[:, 269:270]
    sh1 = mod0[:, 0:16]
    gt1 = mod0[:, 32:48]
    sh2 = mod0[:, 48:64]
    gt2 = mod0[:, 80:96]
    if "mod0" in debug:
        d_ = dbg_out("mod0", [128, 96])
        S.dma("sp", d_[:, :], mod0[:, :], r=[mod0], w=[d_])
        d_ = dbg_out("kvm", [128, 32])
        S.dma("sp", d_[:, :], kvm[:, :], r=[kvm], w=[d_])
    if stop_after == "mod":
        SC.pop()
        return nc, dbg

    SC.push()
    qT = S.sb("qT", [128, 16, 1024], BF16)
    kT = S.sb("kT", [128, 4, 2048], BF16)
    Vs = S.sb("Vs", [128, 16, 512], BF16)
    qiT = S.sb("qiT", [128, 8, 1024], BF16)
    kiT2 = S.sb("kiT2", [128, 2048], BF16)
    wis = S.sb("wis", [128, 8, 16], F32)
    MOFF = [0]
    for j in range(8):
        MOFF.append(MOFF[-1] + 2 * (j + 1) * 128)
    mneg = S.sb("mneg", [128, MOFF[-1]], BF16)

    SC.push()
    hT = S.sb("hT", [128, 16, 2048], BF16)
    SC.push()
    xt = [S.sb(f"xt{i}", [128, 2048], F32) for i in range(2)]
    xn = [S.sb(f"xn{i}", [128, 2048], BF16) for i in range(2)]
    junk = S.sb("junk", [128, 2048], BF16)
    ss = S.sb("ss", [128, 16], F32)
    rs = S.sb("rs", [128, 16], F32)
    ptr = [S.ps(f"ptr{i}", [128, 8, 128], BF16) for i in range(4)]
    n = 0
    for tb in range(16):
        x_ = xt[tb % 2]
        n_ = xn[tb % 2]
        S.dma(WQ[tb % 2], x_[:, :], x_all[tb * 128:(tb + 1) * 128, :], r=[x_all], w=[x_])
        S.act(junk[:, :], x_[:, :], AF.Square, accum_out=ss[:, tb:tb + 1], r=[x_], w=[junk, ss.k(tb)])
        S.act(rs[:, tb:tb + 1], ss[:, tb:tb + 1], AF.Sqrt, scale=1.0 / 2048, bias=epsc,
              r=[ss.k(tb)], w=[rs.k(tb)])
        S.v("dve", "reciprocal", rs[:, tb:tb + 1], rs[:, tb:tb + 1], r=[rs.k(tb)], w=[rs.k(tb)])
        S.v("dve", "tensor_scalar", n_[:, :], x_[:, :], rs[:, tb:tb + 1], None, ALU.mult,
            r=[x_, rs.k(tb)], w=[n_])
        for g in range(2):
            p_ = ptr[n % 4]
            n += 1
            for c8 in range(8):
                c = g * 8 + c8
                S.tr(p_[:, c8, :], n_[:, c * 128:(c + 1) * 128], identb[:, :], r=[n_, identb], w=[p_])
            for c8 in range(8):
                c = g * 8 + c8
                if c8 % 2 == 0:
                    S.act(hT[:, c, tb * 128:(tb + 1) * 128], p_[:, c8, :], AF.Identity,
                          bias=sh1[:, c:c + 1], scale=s1[:, c:c + 1], r=[p_, mod0, s1], w=[hT.k(tb)])
                else:
                    S.v("dve", "tensor_scalar", hT[:, c, tb * 128:(tb + 1) * 128], p_[:, c8, :],
                        s1[:, c:c + 1], sh1[:, c:c + 1], ALU.mult, ALU.add, r=[p_, mod0, s1], w=[hT.k(tb)])
    if "hT" in debug:
        d_ = dbg_out("hT", [128, 16 * 2048], BF16)
        S.dma("sp", d_[:, :].rearrange("p (a b) -> p a b", a=16), hT[:, :, :], r=[hT.k(tb) for tb in range(16)], w=[d_])
    SC.pop()
    if stop_after == "A1":
        SC.pop(); SC.pop(); SC.pop()
        return nc, dbg

    SC.push()
    wt_ = [S.sb(f"w{i}", [128, 16, 256], BF16) for i in range(3)]
    sqb = [S.sb(f"sqb{i}", [128, 512], BF16) for i in range(2)]
    rsd = [S.sb(f"rsd{i}", [128, 512], F32) for i in range(2)]
    pp = [S.ps(f"pp{i}", [128, 512], F32) for i in range(3)]
    pn = [S.ps(f"pn{i}", [128, 512], F32) for i in range(2)]
    hall = [hT.k(tb) for tb in range(16)]
    cnt = {"w": 0, "p": 0, "n": 0}

    def next_w():
        t = wt_[cnt["w"] % 3]
        cnt["w"] += 1
        return t

    def headnorm(p_, ones_, gcol, out_ap, out_buf):
        i = cnt["n"]
        cnt["n"] += 1
        q_ = sqb[i % 2]
        r_ = rsd[i % 2]
        n_ = pn[i % 2]
        S.act(q_[:, :], p_[:, :], AF.Square, r=[p_], w=[q_])
        S.mm(n_[:, :], ones_[:, :], q_[:, :], r=[ones_, q_], w=[n_])
        return q_, r_, n_

    def proj_fm(cols0, ncols_per, gcol, ones_, nrm_div, dst_fn, ntok):
        pass

    def fm_heads(col0, nheads, ntok, dstbuf, gcol, normalize, ones_, div):
        for hp in range(nheads // 2):
            w_ = next_w()
            wload(S, "pool", w_, w_[:, :, :], a_w_in, col0 + hp * 256, 256)
            for hh in range(2):
                h = hp * 2 + hh
                for q4 in range(ntok // 512):
                    p_ = pp[cnt["p"] % 3]
                    cnt["p"] += 1
                    ts_ = slice(q4 * 512, (q4 + 1) * 512)
                    for kc in range(16):
                        S.mm(p_[:, :], w_[:, kc, hh * 128:(hh + 1) * 128], hT[:, kc, ts_],
                             start=(kc == 0), stop=(kc == 15), r=[w_] + hall, w=[p_])
                    if normalize:
                        i = cnt["n"]
                        cnt["n"] += 1
                        q_ = sqb[i % 2]
                        r_ = rsd[i % 2]
                        n_ = pn[i % 2]
                        S.act(q_[:, :], p_[:, :], AF.Square, r=[p_], w=[q_])
                        S.mm(n_[:, :], ones_[:, :], q_[:, :], r=[ones_, q_], w=[n_])
                        S.act(r_[:, :], n_[:, :], AF.Sqrt, scale=1.0 / div, bias=epsc, r=[n_], w=[r_])
                        S.v("dve", "reciprocal", r_[:, :], r_[:, :], r=[r_], w=[r_])
                        S.v("dve", "scalar_tensor_tensor", dstbuf[:, h, ts_], p_[:, :], gcol, r_[:, :],
                            ALU.mult, ALU.mult, r=[p_, r_, gs, gq], w=[dstbuf])
                    else:
                        S.act(dstbuf[:, h, ts_], p_[:, :], AF.Copy, r=[p_], w=[dstbuf])

    fm_heads(0, 16, 1024, qT, gq[:, 0:1], True, onesb, 128.0)
    fm_heads(2048, 4, 2048, kT, gs[:, 1:2], True, onesb, 128.0)
    fm_heads(3072, 8, 1024, qiT, None, False, None, None)
    w_ = next_w()
    S.dma("pool", w_[:, :, 0:64], a_w_in[:, 4096:4160].rearrange("(k p) n -> p k n", p=128), r=[a_w_in], w=[w_])
    S.dma("pool", w_[:, :, 64:128], a_w_in[:, 4096:4160].rearrange("(k p) n -> p k n", p=128), r=[a_w_in], w=[w_])
    S.dma("pool", w_[:, :, 128:144], a_w_in[:, 4160:4176].rearrange("(k p) n -> p k n", p=128), r=[a_w_in], w=[w_])
    for q4 in range(4):
        p_ = pp[cnt["p"] % 3]
        cnt["p"] += 1
        ts_ = slice(q4 * 512, (q4 + 1) * 512)
        for kc in range(16):
            S.mm(p_[:, :], w_[:, kc, 0:128], hT[:, kc, ts_], start=(kc == 0), stop=(kc == 15),
                 r=[w_] + hall, w=[p_])
        S.act(kiT2[:, ts_], p_[:, :], AF.Copy, r=[p_], w=[kiT2])
    for tb in range(8):
        p_ = pp[cnt["p"] % 3]
        cnt["p"] += 1
        for kc in range(16):
            S.mm(p_[:, 0:16], hT[:, kc, tb * 128:(tb + 1) * 128], w_[:, kc, 128:144],
                 start=(kc == 0), stop=(kc == 15), r=[w_] + hall, w=[p_])
        S.v("dve", "tensor_scalar", wis[:, tb, :], p_[:, 0:16], (64.0 ** -0.5) * (16.0 ** -0.5), None, ALU.mult,
            r=[p_], w=[wis])
    for hp in range(2):
        w_ = next_w()
        wload(S, "pool", w_, w_[:, :, :], a_w_in, 2560 + hp * 256, 256)
        for tb in range(16):
            p_ = pp[cnt["p"] % 3]
            cnt["p"] += 1
            for kc in range(16):
                S.mm(p_[:, 0:256], hT[:, kc, tb * 128:(tb + 1) * 128], w_[:, kc, :],
                     start=(kc == 0), stop=(kc == 15), r=[w_] + hall, w=[p_])
            if tb % 2 == 0:
                S.act(Vs[:, tb, hp * 256:(hp + 1) * 256], p_[:, 0:256], AF.Copy, r=[p_], w=[Vs])
            else:
                S.v("dve", "tensor_copy", Vs[:, tb, hp * 256:(hp + 1) * 256], p_[:, 0:256], r=[p_], w=[Vs])
    for nm, b_, shp in (("qT", qT, [128, 16 * 1024]), ("kT", kT, [128, 4 * 2048]), ("Vs", Vs, [128, 16 * 512]),
                        ("qiT", qiT, [128, 8 * 1024]), ("kiT2", kiT2, [128, 2048])):
        if nm in debug:
            d_ = dbg_out(nm, shp, BF16)
            if nm == "kiT2":
                S.dma("sp", d_[:, :], b_[:, :], r=[b_], w=[d_])
            else:
                S.dma("sp", d_[:, :].rearrange("p (a b) -> p a b", a=b_.t.shape[1]), b_[:, :, :], r=[b_], w=[d_])
    if "wis" in debug:
        d_ = dbg_out("wis", [128, 128])
        S.dma("sp", d_[:, :].rearrange("p (a b) -> p a b", a=8), wis[:, :, :], r=[wis], w=[d_])
    SC.pop()
    SC.pop()
    if stop_after == "A2":
        SC.pop(); SC.pop()
        return nc, dbg
    SC.push()
    kidxb = S.sb("kidxb", [128, 2048], F32)
    S.dma("sp", kidxb[:, :], kidx[:, :], r=[kidx], w=[kidxb])
    isc = [S.sb(f"isc{i}", [128, 2048], F32) for i in range(2)]
    work = S.sb("work", [128, 2048], F32)
    rr = [S.sb(f"rr{i}", [128, 512], F32) for i in range(3)]
    m8 = [S.sb(f"m8{i}", [128, 8], F32) for i in range(2)]
    pen = [S.sb(f"pen{i}", [128, 128], F32) for i in range(2)]
    pi = [S.ps(f"pi{i}", [128, 512], F32) for i in range(3)]
    npi = 0
    for j in range(8):
        W_ = (j + 1) * 128
        L = 2 * W_
        ic = isc[j % 2]
        for rng in range(2):
            for p0 in range(0, W_, 512):
                pw = min(512, W_ - p0)
                sc0 = rng * 1024 + p0
                ic0 = rng * W_ + p0
                for h in range(16):
                    pr_, hf = h // 2, h % 2
                    ps_ = slice(64 * hf, 64 * hf + 64)
                    p_ = pi[npi % 3]
                    r_ = rr[npi % 3]
                    npi += 1
                    S.mm(p_[:, 0:pw], qiT[ps_, pr_, j * 128:(j + 1) * 128], kiT2[ps_, sc0:sc0 + pw], w=[p_])
                    S.act(r_[:, 0:pw], p_[:, 0:pw], AF.Relu, r=[p_], w=[r_])
                    if h == 0:
                        S.v("dve", "tensor_scalar", ic[:, ic0:ic0 + pw], r_[:, 0:pw], wis[:, j, 0:1], None, ALU.mult,
                            r=[r_], w=[ic])
                    else:
                        S.v("dve", "scalar_tensor_tensor", ic[:, ic0:ic0 + pw], r_[:, 0:pw], wis[:, j, h:h + 1],
                            ic[:, ic0:ic0 + pw], ALU.mult, ALU.add, r=[r_, ic], w=[ic])
        for rng in range(2):
            pn_ = pen[rng]
            sc0 = rng * 1024 + j * 128
            ic0 = rng * W_ + j * 128
            S.v("dve", "tensor_scalar", pn_[:, :], kidxb[:, sc0:sc0 + 128], cf[:, 256 + j:257 + j], BIGNEG,
                ALU.is_gt, ALU.mult, r=[kidxb], w=[pn_])
            S.v("dve", "tensor_tensor", ic[:, ic0:ic0 + 128], ic[:, ic0:ic0 + 128], pn_[:, :], ALU.add,
                r=[ic, pn_], w=[ic])
        if "isc" in debug and j == 3:
            d_ = dbg_out("isc", [128, 2048])
            S.dma("sp", d_[:, :], ic[:, :], r=[ic], w=[d_])
        mo = MOFF[j]
        if j == 0:
            S.v("dve", "tensor_scalar", mneg[:, mo:mo + L], ic[:, 0:L], -1.0e29, NEG, ALU.is_lt, ALU.mult,
                r=[ic], w=[mneg])
        else:
            mm_ = m8[0]
            S.v("dve", "max", mm_[:, :], ic[:, 0:L], r=[ic], w=[mm_])
            S.v("dve", "match_replace", work[:, 0:L], mm_[:, :], ic[:, 0:L], BIGNEG, r=[ic, mm_], w=[work])
            for rd in range(1, 32):
                mm_ = m8[rd % 2]
                S.v("dve", "max", mm_[:, :], work[:, 0:L], r=[work], w=[mm_])
                if rd < 31:
                    S.v("dve", "match_replace", work[:, 0:L], mm_[:, :], work[:, 0:L], BIGNEG, r=[work, mm_], w=[work])
            S.v("dve", "tensor_scalar", mneg[:, mo:mo + L], ic[:, 0:L], mm_[:, 7:8], NEG, ALU.is_lt, ALU.mult,
                r=[ic, mm_], w=[mneg])
    if "mneg" in debug:
        d_ = dbg_out("mneg", [128, MOFF[-1]], BF16)
        S.dma("sp", d_[:, :], mneg[:, :], r=[mneg], w=[d_])
    SC.pop()
    if stop_after == "idx":
        SC.pop(); SC.pop()
        return nc, dbg

    SC.push()
    Gt = [S.sb("Gown", [128, 16, 8, 128], BF16), S.sb("Gpar", [128, 16, 8, 128], BF16)]
    SC.push()
    relr = S.sb("relr", [32, 2304], F32)
    nn_ = S.sb("nn_", [32, 2304], F32)
    aa_ = S.sb("aa_", [32, 2304], F32)
    relbs = S.sb("relbs", [32, 16], F32)
    frow = S.sb("frow", [16, 2304], BF16)
    pfr = [S.ps(f"pfr{i}", [16, 512], F32) for i in range(2)]
    S.dma("sp", relr[:, :], relrow[:, :], r=[relrow], w=[relr])
    S.dma("sp", relbs[:, :], relb[:, :], r=[relb], w=[relbs])
    for kind in range(2):
        if kind == 0:
            S.v("dve", "tensor_scalar", nn_[:, :], relr[:, :], 0.0, None, ALU.max, r=[relr], w=[nn_])
        else:
            S.v("dve", "tensor_scalar", nn_[:, :], relr[:, :], cf[0:32, 264:265], 0.0, ALU.add, ALU.max,
                r=[relr], w=[nn_])
        S.v("dve", "tensor_scalar", aa_[:, :], nn_[:, :], cf[0:32, 265:266], None, ALU.is_ge, r=[nn_], w=[aa_])
        S.v("dve", "scalar_tensor_tensor", aa_[:, :], nn_[:, :], cf[0:32, 266:267], aa_[:, :], ALU.is_lt, ALU.mult,
            r=[nn_, aa_], w=[aa_])
        for pc in range(5):
            c0 = pc * 512
            pw = min(512, 2304 - c0)
            p_ = pfr[pc % 2]
            S.mm(p_[:, 0:pw], relbs[:, :], aa_[:, c0:c0 + pw], r=[relbs, aa_], w=[p_])
            S.act(frow[:, c0:c0 + pw], p_[:, 0:pw], AF.Copy, r=[p_], w=[frow])
        S.dma("sp", frowD[kind * 16:(kind + 1) * 16, :], frow[:, :], r=[frow], w=[frowD])
    for kind in range(2):
        for h in range(16):
            src = bass.AP(frowD.t.tensor, (kind * 16 + h) * 2304 + 129, [[1, 128], [256, 8], [1, 128]])
            S.dma(WQ[h % 2], Gt[kind][:, h, :, :], src, r=[frowD], w=[Gt[kind]])
    SC.pop()
    if "G" in debug:
        d_ = dbg_out("G", [128, 2 * 16 * 8 * 128], BF16)
        for kind in range(2):
            S.dma("sp", d_[:, kind * 16384:(kind + 1) * 16384].rearrange("p (h t q) -> p h t q", h=16, t=8),
                  Gt[kind][:, :, :, :], r=[Gt[kind]], w=[d_])

    SC.push()
    pT = [S.sb(f"pT{i}", [128, 4, 128], BF16) for i in range(3)]
    rden = [S.sb(f"rden{i}", [128, 128], F32) for i in range(2)]
    ostage = [S.sb(f"ostage{i}", [128, 16, 128], BF16) for i in range(2)]
    st = [S.ps(f"st{i}", [128, 4, 128], F32) for i in range(3)]
    po = [S.ps(f"po{i}", [128, 512], F32) for i in range(2)]
    pd = [S.ps(f"pd{i}", [128, 512], F32) for i in range(2)]
    oT_dv = oT_d[:, :].rearrange("p (h t) -> p h t", h=16)
    nst = 0
    nh = 0
    for j in range(8):
        blocks = [(0, m) for m in range(j + 1)] + [(1, m) for m in range(j + 1)]
        nblk = len(blocks)
        og = ostage[j % 2]
        for h in range(16):
            kvh = h // 4
            po_ = po[nh % 2]
            pd_ = pd[nh % 2]
            rd_ = rden[nh % 2]
            nh += 1
            for g0 in range(0, nblk, 4):
                grp = blocks[g0:g0 + 4]
                st_ = st[nst % 3]
                pT_ = pT[nst % 3]
                nst += 1
                for ii, (kind, m) in enumerate(grp):
                    i = g0 + ii
                    sb_ = kind * 8 + m
                    S.mm(st_[:, ii, :], kT[:, kvh, sb_ * 128:(sb_ + 1) * 128], qT[:, h, j * 128:(j + 1) * 128],
                         start=True, stop=False, w=[st_])
                    S.mm(st_[:, ii, :], mneg[:, MOFF[j] + i * 128:MOFF[j] + (i + 1) * 128], identb[:, :],
                         start=False, stop=False, w=[st_])
                    S.mm(st_[:, ii, :], antib[:, :], Gt[kind][:, h, j - m, :], start=False, stop=True,
                         r=[Gt[kind]], w=[st_])
                ng = len(grp)
                S.act(pT_[:, 0:ng, :], st_[:, 0:ng, :], AF.Exp, r=[st_], w=[pT_])
                for ii, (kind, m) in enumerate(grp):
                    i = g0 + ii
                    sb_ = kind * 8 + m
                    S.mm(po_[:, 0:128], Vs[:, sb_, kvh * 128:(kvh + 1) * 128], pT_[:, ii, :],
                         start=(i == 0), stop=(i == nblk - 1), r=[pT_], w=[po_], sig=False)
                    S.mm(pd_[:, 0:128], onesb[:, :], pT_[:, ii, :],
                         start=(i == 0), stop=(i == nblk - 1), r=[pT_], w=[pd_], sig=(ii == ng - 1))
            S.v("dve", "reciprocal", rd_[:, :], pd_[:, 0:128], r=[pd_], w=[rd_])
            S.v("dve", "tensor_tensor", og[:, h, :], po_[:, 0:128], rd_[:, :], ALU.mult, r=[po_, pd_, rd_], w=[og])
        S.dma("sp", oT_dv[:, :, j * 128:(j + 1) * 128], og[:, :, :], r=[og], w=[oT_d])
    SC.pop()
    SC.pop()
    SC.pop()
    if stop_after == "att":
        if "oT" in debug:
            SC.push()
            tmpo = S.sb("tmpo", [128, 16 * 1024], BF16)
            d_ = dbg_out("oT", [128, 16 * 1024], BF16)
            S.dma("sp", tmpo[:, :], oT_d[:, :], r=[oT_d], w=[tmpo])
            S.dma("sp", d_[:, :], tmpo[:, :], r=[tmpo], w=[d_])
            SC.pop()
        SC.pop()
        return nc, dbg
    SC.push()
    xT = S.sb("xT", [128, 16, 1024], F32)
    h2T = S.sb("h2T", [128, 16, 1024], BF16)
    rstd = S.sb("rstd", [128, 1024], F32)
    SC.push()
    oT = S.sb("oT", [128, 16, 1024], BF16)
    wt_ = [S.sb(f"wo{i}", [128, 16, 256], BF16) for i in range(2)]
    xt = [S.sb(f"xtb{i}", [128, 2048], F32) for i in range(2)]
    ptx = [S.ps(f"ptx{i}", [128, 4, 128], F32) for i in range(2)]
    pa = [S.ps(f"pa{i}", [128, 512], F32) for i in range(3)]
    S.dma("sp", oT[:, :, :], oT_d[:, :].rearrange("p (h t) -> p h t", h=16), r=[oT_d], w=[oT])
    n = 0
    for tb in range(8):
        x_ = xt[tb % 2]
        S.dma(WQ[tb % 2], x_[:, :], x_all[tb * 128:(tb + 1) * 128, :], r=[x_all], w=[x_])
        for c4 in range(4):
            p_ = ptx[n % 2]
            n += 1
            for ci in range(4):
                c = c4 * 4 + ci
                S.tr(p_[:, ci, :], x_[:, c * 128:(c + 1) * 128], cf[:, 0:128], r=[x_], w=[p_])
            if c4 % 2 == 0:
                S.v("dve", "tensor_copy", xT[:, c4 * 4:c4 * 4 + 4, tb * 128:(tb + 1) * 128], p_[:, :, :], r=[p_], w=[xT.k(tb // 4)])
            else:
                S.act(xT[:, c4 * 4:c4 * 4 + 4, tb * 128:(tb + 1) * 128], p_[:, :, :], AF.Copy, r=[p_], w=[xT.k(tb // 4)])
    n = 0
    for fcg in range(8):
        w_ = wt_[fcg % 2]
        wload(S, "pool", w_, w_[:, :, :], a_w_out, fcg * 256, 256)
        for fc2 in range(2):
            fc = fcg * 2 + fc2
            for half in range(2):
                p_ = pa[n % 3]
                n += 1
                hs = slice(half * 512, (half + 1) * 512)
                for h in range(16):
                    S.mm(p_[:, :], w_[:, h, fc2 * 128:(fc2 + 1) * 128], oT[:, h, hs], start=(h == 0), stop=(h == 15),
                         r=[w_, oT], w=[p_])
                S.v("dve", "scalar_tensor_tensor", xT[:, fc, hs], p_[:, :], gt1[:, fc:fc + 1], xT[:, fc, hs],
                    ALU.mult, ALU.add, r=[p_, xT.k(half)], w=[xT.k(half)])
    SC.pop()
    if "xa" in debug:
        d_ = dbg_out("xa", [128, 16 * 1024])
        S.dma("sp", d_[:, :].rearrange("p (a b) -> p a b", a=16), xT[:, :, :], r=[xT.k(0), xT.k(1)], w=[d_])
    if stop_after == "wout":
        SC.pop(); SC.pop()
        return nc, dbg

    def norm_phase(scol, bcol, tag=[0]):
        SC.push()
        tag[0] += 1
        sq = [S.sb(f"sq{i}_{tag[0]}", [128, 512], BF16) for i in range(2)]
        tmp = [S.sb(f"tmpn{i}_{tag[0]}", [128, 512], F32) for i in range(2)]
        pss = [S.ps(f"pss{i}_{tag[0]}", [128, 512], F32) for i in range(2)]
        xall = [xT.k(0), xT.k(1)]
        for half in range(2):
            cs_ = slice(half * 512, (half + 1) * 512)
            ps_ = pss[half]
            for c in range(16):
                q = sq[c % 2]
                S.act(q[:, :], xT[:, c, cs_], AF.Square, r=xall, w=[q])
                S.mm(ps_[:, :], onesb[:, :], q[:, :], start=(c == 0), stop=(c == 15), r=[q], w=[ps_])
            S.act(rstd[:, cs_], ps_[:, :], AF.Sqrt, scale=1.0 / 2048, bias=epsc, r=[ps_], w=[rstd])
            S.v("dve", "reciprocal", rstd[:, cs_], rstd[:, cs_], r=[rstd], w=[rstd])
            for c in range(16):
                t = tmp[c % 2]
                S.v("dve", "scalar_tensor_tensor", t[:, :], xT[:, c, cs_], scol[:, c:c + 1], rstd[:, cs_],
                    ALU.mult, ALU.mult, r=xall + [rstd], w=[t])
                S.act(h2T[:, c, cs_], t[:, :], AF.Identity, bias=bcol[:, c:c + 1], scale=1.0, r=[t], w=[h2T])
        SC.pop()

    norm_phase(s2, sh2)
    if "h2" in debug:
        d_ = dbg_out("h2", [128, 16 * 1024], BF16)
        S.dma("sp", d_[:, :].rearrange("p (a b) -> p a b", a=16), h2T[:, :, :], r=[h2T], w=[d_])
    if stop_after == "norm2":
        SC.pop(); SC.pop()
        return nc, dbg
    SC.push()
    w1t = [S.sb(f"w1t{i}", [128, 16, 256], BF16) for i in range(3)]
    w3t = [S.sb(f"w3t{i}", [128, 16, 256], BF16) for i in range(3)]
    w2t = [S.sb(f"w2t{i}", [128, 2048], BF16) for i in range(5)]
    ug = [S.sb(f"ug{i}", [128, 4, 1024], BF16) for i in range(2)]
    sil = [S.sb(f"sil{i}", [128, 512], BF16) for i in range(2)]
    p1 = [S.ps(f"p1{i}", [128, 512], F32) for i in range(2)]
    p3 = [S.ps(f"p3{i}", [128, 512], F32) for i in range(2)]
    py = [S.ps(f"py{i}", [128, 512], F32) for i in range(3)]
    n13 = 0
    ny = 0
    nw = 0
    nw2 = 0
    for g in range(11):
        u_ = ug[g % 2]
        a1s, a3s, a2s = [], [], []
        for t2 in range(2):
            a1, a3 = w1t[nw % 3], w3t[nw % 3]
            nw += 1
            wload(S, "pool", a1, a1[:, :, :], ffn_w1, g * 512 + t2 * 256, 256)
            wload(S, "pool", a3, a3[:, :, :], ffn_w3, g * 512 + t2 * 256, 256)
            a1s.append(a1)
            a3s.append(a3)
        for ci in range(4):
            a2 = w2t[nw2 % 5]
            nw2 += 1
            r0 = g * 512 + ci * 128
            for hh in range(2):
                S.dma("pool", a2[:, hh * 1024:(hh + 1) * 1024],
                      ffn_w2[r0:r0 + 128, hh * 1024:(hh + 1) * 1024], r=[ffn_w2], w=[a2])
            a2s.append(a2)
        for ci in range(4):
            a1, a3 = a1s[ci // 2], a3s[ci // 2]
            c = ci % 2
            for half in range(2):
                hs = slice(half * 512, (half + 1) * 512)
                q1, q3, sl = p1[n13 % 2], p3[n13 % 2], sil[n13 % 2]
                n13 += 1
                for kc in range(16):
                    S.mm(q1[:, :], a1[:, kc, c * 128:(c + 1) * 128], h2T[:, kc, hs], start=(kc == 0), stop=(kc == 15),
                         r=[a1, h2T], w=[q1])
                for kc in range(16):
                    S.mm(q3[:, :], a3[:, kc, c * 128:(c + 1) * 128], h2T[:, kc, hs], start=(kc == 0), stop=(kc == 15),
                         r=[a3, h2T], w=[q3])
                S.act(sl[:, :], q1[:, :], AF.Silu, r=[q1], w=[sl])
                S.v("dve", "tensor_tensor", u_[:, ci, hs], sl[:, :], q3[:, :], ALU.mult, r=[sl, q3], w=[u_])
        for fc in range(16):
            for half in range(2):
                hs = slice(half * 512, (half + 1) * 512)
                y_ = py[ny % 3]
                ny += 1
                for ci in range(4):
                    S.mm(y_[:, :], a2s[ci][:, fc * 128:(fc + 1) * 128], u_[:, ci, hs], start=(ci == 0), stop=(ci == 3),
                         r=[a2s[ci], u_], w=[y_])
                S.v("dve", "scalar_tensor_tensor", xT[:, fc, hs], y_[:, :], gt2[:, fc:fc + 1], xT[:, fc, hs],
                    ALU.mult, ALU.add, r=[y_, xT.k(half)], w=[xT.k(half)])
    SC.pop()
    S.dma("sp", x1_out[:, :].rearrange("p (a b) -> p a b", a=16), xT[:, :, :], r=[xT.k(0), xT.k(1)], w=[x1_out])
    if stop_after == "ffn":
        SC.pop(); SC.pop()
        return nc, dbg

    norm_phase(skv, kvm)
    SC.push()
    wk_ = [S.sb(f"wk{i}", [128, 16, 256], BF16) for i in range(2)]
    sqb = [S.sb(f"ksq{i}", [128, 512], BF16) for i in range(2)]
    rsd = [S.sb(f"krs{i}", [128, 512], F32) for i in range(2)]
    kt_ = [S.sb(f"kt{i}", [128, 512], BF16) for i in range(3)]
    vt_ = [S.sb(f"vt{i}", [128, 256], BF16) for i in range(3)]
    pk = [S.ps(f"pk{i}", [128, 512], F32) for i in range(3)]
    pn = [S.ps(f"pkn{i}", [128, 512], F32) for i in range(2)]
    kT_ov = kT_out[:, :].rearrange("p (a b) -> p a b", a=24)
    n = 0
    for g2 in range(12):
        w_ = wk_[g2 % 2]
        wload(S, "pool", w_, w_[:, :, :], kv_w, g2 * 256, 256)
        for c in range(2):
            ptile = g2 * 2 + c
            for half in range(2):
                hs = slice(half * 512, (half + 1) * 512)
                p_, q_, r_, n_, k_ = pk[n % 3], sqb[n % 2], rsd[n % 2], pn[n % 2], kt_[n % 3]
                n += 1
                for kc in range(16):
                    S.mm(p_[:, :], w_[:, kc, c * 128:(c + 1) * 128], h2T[:, kc, hs], start=(kc == 0), stop=(kc == 15),
                         r=[w_, h2T], w=[p_])
                S.act(q_[:, :], p_[:, :], AF.Square, r=[p_], w=[q_])
                S.mm(n_[:, :], bdones[:, :], q_[:, :], r=[q_], w=[n_])
                S.act(r_[:, :], n_[:, :], AF.Sqrt, scale=1.0 / 64, bias=epsc, r=[n_], w=[r_])
                S.v("dve", "reciprocal", r_[:, :], r_[:, :], r=[r_], w=[r_])
                S.v("dve", "scalar_tensor_tensor", k_[:, :], p_[:, :], gs[:, 2:3], r_[:, :], ALU.mult, ALU.mult,
                    r=[p_, r_], w=[k_])
                S.dma("sp", kT_ov[:, ptile, hs], k_[:, :], r=[k_], w=[kT_out])
                if ctx is not None:
                    S.dma("act", KT_allv[:, ptile, hs], k_[:, :], r=[k_], w=[KT_all])
    n = 0
    for g2 in range(12):
        w_ = wk_[g2 % 2]
        wload(S, "pool", w_, w_[:, :, :], kv_w, 3072 + g2 * 256, 256)
        for tb in range(8):
            p_, v_ = pk[n % 3], vt_[n % 3]
            n += 1
            for kc in range(16):
                S.mm(p_[:, 0:256], h2T[:, kc, tb * 128:(tb + 1) * 128], w_[:, kc, :], start=(kc == 0), stop=(kc == 15),
                     r=[w_, h2T], w=[p_])
            if tb % 2 == 0:
                S.act(v_[:, :], p_[:, 0:256], AF.Copy, r=[p_], w=[v_])
            else:
                S.v("dve", "tensor_copy", v_[:, :], p_[:, 0:256], r=[p_], w=[v_])
            S.dma("sp", v_out[tb * 128:(tb + 1) * 128, g2 * 256:(g2 + 1) * 256], v_[:, :], r=[v_], w=[v_out])
            if ctx is not None:
                S.dma("act", V_all[tb * 128:(tb + 1) * 128, g2 * 256:(g2 + 1) * 256], v_[:, :], r=[v_], w=[V_all])
    SC.pop()
    SC.pop()
    SC.pop()
    return nc, dbg


def fm16(v):
    return np.ascontiguousarray(np.asarray(v, np.float32).reshape(16, 128).T)


def prep_A(inp):
    maps = []
    for core in range(8):
        b, par = core // 2, core % 2
        order = storage_order(par)
        xb = inp["x"][b].reshape(16, 128, 2048)[order].reshape(2048, 2048)
        gsm = np.zeros((128, 4), np.float32)
        gsm[:, 0] = inp["a_g_qn"][0]
        gsm[:, 1] = inp["a_g_kn"][0]
        gsm[:, 2] = np.tile(inp["b_g_kn"], 2)
        m = {
            "x_all": np.ascontiguousarray(xb),
            "c_fm": fm16(inp["c"][b]),
            "consts": make_consts(par),
            "kidx": np.ascontiguousarray(np.broadcast_to(make_kidx(par), (128, 2048))),
            "relrow": make_relrow(),
            "rel_bias": np.ascontiguousarray(inp["rel_bias"]),
            "gv": np.concatenate([fm16(inp["g_attn"][0]), fm16(inp["g_ffn"][0]), fm16(inp["kv_g"])], axis=1),
            "gsm": gsm,
            "w_mod0": inp["w_mod"][0], "b_mod0": inp["b_mod"][0][None, :],
            "kv_w_mod": inp["kv_w_mod"], "kv_b_mod": inp["kv_b_mod"][None, :],
            "a_w_in": inp["a_w_in"][0], "a_w_out": inp["a_w_out"][0],
            "ffn_w1": inp["ffn_w1"][0], "ffn_w3": inp["ffn_w3"][0], "ffn_w2": inp["ffn_w2"][0],
            "kv_w": inp["kv_w"],
        }
        maps.append(m)
    return maps


L1_WIN = ((128, 1), (512, 4), (2048, 16))


def l1_tiles():
    TL = []
    for g, (win, r) in enumerate(L1_WIN):
        for kind in range(2):
            for t in range(8):
                ok = False
                for par in range(2):
                    off = 128 * (2 * par - 1) if kind == 1 else 0
                    lo = 256 * t + off - 127
                    hi = 256 * t + off + 127
                    if hi >= 0 and lo <= win:
                        ok = True
                if ok:
                    TL.append((g, kind, t))
    return TL


def make_l1_masks(parity):
    TL = l1_tiles()
    k = np.arange(128)[:, None]
    q = np.arange(128)[None, :]
    m = np.zeros((128, len(TL), 128), np.float32)
    for i, (g, kind, t) in enumerate(TL):
        win, r = L1_WIN[g]
        off = 128 * (2 * parity - 1) if kind == 1 else 0
        rel = 256 * t + off + q - k
        ok = (rel >= 0) & (rel <= win) & (rel % r == 0)
        m[:, i, :] = np.where(ok, 0.0, NEG)
    return np.ascontiguousarray(m.reshape(128, -1))


def build_B(stop_after=None, debug=(), dense_experts=True, ctx=None):
    if ctx is None:
        nc = bass.Bass("TRN2", target_bir_lowering=False)
        S = Sched(nc)
        SC = Scope(S)
    else:
        nc, S, SC = ctx["nc"], ctx["S"], ctx["SC"]
    S.prefix = "zb_"
    dbg = {}
    TL = l1_tiles()
    NT = len(TL)

    def ext(name, shape, dt=F32):
        return S.dram(name, shape, dt, kind="ExternalInput")

    if ctx is None:
        x1fm = ext("x1fm", [128, 16 * 1024])
        KT_all = ext("KT_all", [128, 24 * 2048], BF16)
        V_all = ext("V_all", [2048, 3072], BF16)
    else:
        x1fm, KT_all, V_all = ctx["x1_out"], ctx["KT_all"], ctx["V_all"]
    c_fm = ext("c_fm", [128, 16])
    consts = ext("consts", [128, NCONST])
    relrow = ext("relrow", [32, 2304])
    relb = ext("rel_bias", [32, 16])
    gv = ext("gvb", [128, 32])
    gsm = ext("gsmb", [128, 4])
    maskl1 = ext("maskl1", [128, NT * 128])
    selc = ext("selc", [8, 8 * 128])
    rb_bc = ext("rb_bc", [128, 8])
    w_mod1 = ext("w_mod1", [2048, 12288])
    b_mod1 = ext("b_mod1", [1, 12288])
    b_w_q = ext("b_w_q", [2048, 3072])
    b_w_out = ext("b_w_out", [1024, 2048])
    router = ext("router", [2048, 8])
    moe_w1 = ext("moe_w1", [8 * 2048, 7168])
    moe_w3 = ext("moe_w3", [8 * 2048, 7168])
    moe_w2 = ext("moe_w2", [8 * 7168, 2048])
    out_fm = S.dram("out_fm", [128, 16 * 1024], F32, kind="ExternalOutput")
    frowD = S.dram("frowD", [2 * 16, 2304], BF16)
    oT_d = S.dram("oT_d", [64, 16 * 1024], BF16)

    def dbg_out(name, shape, dt=F32):
        b = S.dram("dbg_" + name, shape, dt, kind="ExternalOutput")
        dbg[name] = b
        return b

    SC.push()
    cf = S.sb("cf", [128, NCONST], F32)
    identb = S.sb("identb", [128, 128], BF16)
    antib = S.sb("antib", [128, 128], BF16)
    bdones = S.sb("bdones", [128, 128], BF16)
    onesb = S.sb("onesb", [128, 128], BF16)
    cs = S.sb("cs", [128, 16], F32)
    mod1 = S.sb("mod1", [128, 96], F32)
    gvs = S.sb("gvs", [128, 32], F32)
    gs = S.sb("gs", [128, 4], F32)
    s1 = S.sb("s1", [128, 16], F32)
    s2 = S.sb("s2", [128, 16], F32)
    gq = S.sb("gq", [128, 1], F32)
    S.dma("sp", cf[:, :], consts[:, :], r=[consts], w=[cf])
    S.dma("pool", identb[:, :], consts[:, 0:128], r=[consts], w=[identb])
    S.dma("pool", bdones[:, :], consts[:, 128:256], r=[consts], w=[bdones])
    S.dma("pool", antib[:, :], consts[:, 272:400], r=[consts], w=[antib])
    S.dma("sp", cs[:, :], c_fm[:, :], r=[c_fm], w=[cs])
    S.dma("sp", gvs[:, :], gv[:, :], r=[gv], w=[gvs])
    S.dma("sp", gs[:, :], gsm[:, :], r=[gsm], w=[gs])
    S.v("dve", "memset", onesb[:, :], 1.0, w=[onesb])
    S.act(cs[:, :], cs[:, :], AF.Silu, r=[cs], w=[cs])
    mod_rows(S, SC, cs, w_mod1, b_mod1, 12288, mod1, cf, "m1")
    S.v("dve", "scalar_tensor_tensor", s1[:, :], mod1[:, 16:32], 1.0, gvs[:, 0:16], ALU.add, ALU.mult,
        r=[mod1, gvs], w=[s1])
    S.v("dve", "scalar_tensor_tensor", s2[:, :], mod1[:, 64:80], 1.0, gvs[:, 16:32], ALU.add, ALU.mult,
        r=[mod1, gvs], w=[s2])
    S.v("dve", "tensor_scalar", gq[:, :], gs[:, 0:1], 64.0 ** -0.5, None, ALU.mult, r=[gs], w=[gq])
    epsc = cf[:, 269:270]
    sh1 = mod1[:, 0:16]
    gt1 = mod1[:, 32:48]
    sh2 = mod1[:, 48:64]
    gt2 = mod1[:, 80:96]
    S.flush()

    SC.push()
    qT1 = S.sb("qT1", [128, 24, 1024], BF16)
    SC.push()
    xT = S.sb("xTa", [128, 16, 1024], F32)
    hT = S.sb("hTa", [128, 16, 1024], BF16)
    rstd = S.sb("rstda", [128, 1024], F32)
    S.dma("sp", xT[:, :, :], x1fm[:, :].rearrange("p (a b) -> p a b", a=16), r=[x1fm], w=[xT])

    def norm_phase(xT_, h_out, rstd_, scol, bcol, tagn, router_w=None, plog=None):
        SC.push()
        sq = [S.sb(f"sq{i}_{tagn}", [128, 512], BF16) for i in range(2)]
        tmp = [S.sb(f"tmpn{i}_{tagn}", [128, 512], F32) for i in range(2)]
        pss = [S.ps(f"pss{i}_{tagn}", [128, 512], F32) for i in range(2)]
        for half in range(2):
            cs_ = slice(half * 512, (half + 1) * 512)
            ps_ = pss[half]
            for c in range(16):
                q = sq[c % 2]
                S.act(q[:, :], xT_[:, c, cs_], AF.Square, r=[xT_], w=[q])
                S.mm(ps_[:, :], onesb[:, :], q[:, :], start=(c == 0), stop=(c == 15), r=[q], w=[ps_])
            S.act(rstd_[:, cs_], ps_[:, :], AF.Sqrt, scale=1.0 / 2048, bias=epsc, r=[ps_], w=[rstd_])
            S.v("dve", "reciprocal", rstd_[:, cs_], rstd_[:, cs_], r=[rstd_], w=[rstd_])
            for c in range(16):
                t = tmp[c % 2]
                S.v("dve", "scalar_tensor_tensor", t[:, :], xT_[:, c, cs_], scol[:, c:c + 1], rstd_[:, cs_],
                    ALU.mult, ALU.mult, r=[xT_, rstd_], w=[t])
                if router_w is None:
                    S.act(h_out[:, c, cs_], t[:, :], AF.Identity, bias=bcol[:, c:c + 1], scale=1.0, r=[t], w=[h_out])
                else:
                    S.v("dve", "tensor_scalar", t[:, :], t[:, :], bcol[:, c:c + 1], None, ALU.add, r=[t], w=[t])
                    S.act(h_out[:, c, cs_], t[:, :], AF.Copy, r=[t], w=[h_out])
                    for b4 in range(4):
                        pl = plog[half * 4 + b4]
                        S.mm(pl[:, 0:8], t[:, b4 * 128:(b4 + 1) * 128], router_w[:, c, :], start=(c == 0),
                             stop=(c == 15), r=[t, router_w], w=[pl])
        SC.pop()

    norm_phase(xT, hT, rstd, s1, sh1, "a")
    SC.push()
    wt_ = [S.sb(f"wq{i}", [128, 16, 256], BF16) for i in range(2)]
    sqb = [S.sb(f"qsq{i}", [128, 512], BF16) for i in range(2)]
    rsd = [S.sb(f"qrs{i}", [128, 512], F32) for i in range(2)]
    pk = [S.ps(f"pq{i}", [128, 512], F32) for i in range(3)]
    pn = [S.ps(f"pqn{i}", [128, 512], F32) for i in range(2)]
    n = 0
    for g2 in range(12):
        w_ = wt_[g2 % 2]
        wload(S, "pool", w_, w_[:, :, :], b_w_q, g2 * 256, 256)
        for c in range(2):
            ptile = g2 * 2 + c
            for half in range(2):
                hs = slice(half * 512, (half + 1) * 512)
                p_, q_, r_, n_ = pk[n % 3], sqb[n % 2], rsd[n % 2], pn[n % 2]
                n += 1
                for kc in range(16):
                    S.mm(p_[:, :], w_[:, kc, c * 128:(c + 1) * 128], hT[:, kc, hs], start=(kc == 0), stop=(kc == 15),
                         r=[w_, hT], w=[p_])
                S.act(q_[:, :], p_[:, :], AF.Square, r=[p_], w=[q_])
                S.mm(n_[:, :], bdones[:, :], q_[:, :], r=[q_], w=[n_])
                S.act(r_[:, :], n_[:, :], AF.Sqrt, scale=1.0 / 64, bias=epsc, r=[n_], w=[r_])
                S.v("dve", "reciprocal", r_[:, :], r_[:, :], r=[r_], w=[r_])
                S.v("dve", "scalar_tensor_tensor", qT1[:, ptile, hs], p_[:, :], gq[:, 0:1], r_[:, :], ALU.mult, ALU.mult,
                    r=[p_, r_], w=[qT1])
    SC.pop()
    SC.pop()
    if "qT1" in debug:
        d_ = dbg_out("qT1", [128, 24 * 1024], BF16)
        S.dma("sp", d_[:, :].rearrange("p (a b) -> p a b", a=24), qT1[:, :, :], r=[qT1], w=[d_])

    SC.push()
    relr = S.sb("relr", [32, 2304], F32)
    nn_ = S.sb("nn_", [32, 2304], F32)
    aa_ = S.sb("aa_", [32, 2304], F32)
    relbs = S.sb("relbs", [32, 16], F32)
    frow = S.sb("frow", [16, 2304], BF16)
    pfr = [S.ps(f"pfr{i}", [16, 512], F32) for i in range(2)]
    S.dma("sp", relr[:, :], relrow[:, :], r=[relrow], w=[relr])
    S.dma("sp", relbs[:, :], relb[:, :], r=[relb], w=[relbs])
    for kind in range(2):
        if kind == 0:
            S.v("dve", "tensor_scalar", nn_[:, :], relr[:, :], 0.0, None, ALU.max, r=[relr], w=[nn_])
        else:
            S.v("dve", "tensor_scalar", nn_[:, :], relr[:, :], cf[0:32, 264:265], 0.0, ALU.add, ALU.max,
                r=[relr], w=[nn_])
        S.v("dve", "tensor_scalar", aa_[:, :], nn_[:, :], cf[0:32, 265:266], None, ALU.is_ge, r=[nn_], w=[aa_])
        S.v("dve", "scalar_tensor_tensor", aa_[:, :], nn_[:, :], cf[0:32, 266:267], aa_[:, :], ALU.is_lt, ALU.mult,
            r=[nn_, aa_], w=[aa_])
        for pc in range(5):
            c0 = pc * 512
            pw = min(512, 2304 - c0)
            p_ = pfr[pc % 2]
            S.mm(p_[:, 0:pw], relbs[:, :], aa_[:, c0:c0 + pw], r=[relbs, aa_], w=[p_])
            S.act(frow[:, c0:c0 + pw], p_[:, 0:pw], AF.Copy, r=[p_], w=[frow])
        S.dma("sp", frowD[kind * 16:(kind + 1) * 16, :], frow[:, :], r=[frow], w=[frowD])
    SC.pop()

    SC.push()
    mk = S.sb("mk", [128, NT, 128], BF16)
    S.dma("pool", mk[:, :, :], maskl1[:, :].rearrange("p (a b) -> p a b", a=NT), r=[maskl1], w=[mk])
    Kt = [[S.sb(f"Kt{b}_{g}", [128, 2048], BF16) for g in range(3)] for b in range(2)]
    Vt = [[S.sb(f"Vt{b}_{g}", [128, 16, 128], BF16) for g in range(3)] for b in range(2)]
    Gp = [S.sb(f"Gp{b}", [128, 2, 16, 128], BF16) for b in range(2)]
    pT = [S.sb(f"pT{i}", [128, 4, 128], BF16) for i in range(3)]
    rden = [S.sb(f"rden{i}", [64, 128], F32) for i in range(2)]
    ostage = [S.sb(f"ostage{i}", [64, 2, 128], BF16) for i in range(3)]
    st = [S.ps(f"st{i}", [128, 4, 128], F32) for i in range(3)]
    po = [S.ps(f"po{i}", [128, 512], F32) for i in range(2)]
    pd = [S.ps(f"pd{i}", [128, 512], F32) for i in range(2)]
    KT_v = KT_all[:, :].rearrange("p (a b) -> p a b", a=24)
    oT_dv = oT_d[:, :].rearrange("p (h t) -> p h t", h=16)
    nst = 0
    nh = 0
    nos = 0
    for pr in range(8):
        b = pr % 2
        for g in range(3):
            S.dma("sp", Kt[b][g][:, :], KT_v[:, g * 8 + pr, :], r=[KT_all], w=[Kt[b][g]])
            S.dma("act", Vt[b][g][:, :, :],
                  V_all[:, g * 1024 + pr * 128:g * 1024 + (pr + 1) * 128].rearrange("(s p) d -> p s d", p=128),
                  r=[V_all], w=[Vt[b][g]])
        for hh in range(2):
            for kind in range(2):
                src = bass.AP(frowD.t.tensor, (kind * 16 + 2 * pr + hh) * 2304 + 129, [[1, 128], [256, 8], [1, 128]])
                S.dma("sp", Gp[b][:, hh, kind * 8:(kind + 1) * 8, :], src, r=[frowD], w=[Gp[b]])
        for j in range(8):
            og = ostage[nos % 3]
            nos += 1
            for hh in range(2):
                ps_ = slice(64 * hh, 64 * hh + 64)
                tiles = [(i, g, kind, t) for i, (g, kind, t) in enumerate(TL) if t <= j]
                nblk = len(tiles)
                po_, pd_, rd_ = po[nh % 2], pd[nh % 2], rden[nh % 2]
                nh += 1
                for g0 in range(0, nblk, 4):
                    grp = tiles[g0:g0 + 4]
                    st_, pT_ = st[nst % 3], pT[nst % 3]
                    nst += 1
                    for ii, (ti, g, kind, t) in enumerate(grp):
                        sb_ = kind * 8 + (j - t)
                        S.mm(st_[:, ii, :], Kt[b][g][ps_, sb_ * 128:(sb_ + 1) * 128],
                             qT1[ps_, g * 8 + pr, j * 128:(j + 1) * 128], start=True, stop=False,
                             r=[Kt[b][g]], w=[st_])
                        S.mm(st_[:, ii, :], antib[:, :], Gp[b][:, hh, kind * 8 + t, :], start=False, stop=False,
                             r=[Gp[b]], w=[st_])
                        S.mm(st_[:, ii, :], identb[:, :], mk[:, ti, :], start=False, stop=True, r=[mk], w=[st_])
                    ng = len(grp)
                    S.act(pT_[:, 0:ng, :], st_[:, 0:ng, :], AF.Exp, r=[st_], w=[pT_])
                    for ii, (ti, g, kind, t) in enumerate(grp):
                        i = g0 + ii
                        sb_ = kind * 8 + (j - t)
                        S.mm(po_[0:64, 0:128], Vt[b][g][:, sb_, hh * 64:(hh + 1) * 64], pT_[:, ii, :],
                             start=(i == 0), stop=(i == nblk - 1), r=[pT_, Vt[b][g]], w=[po_], sig=False)
                        S.mm(pd_[0:64, 0:128], onesb[:, 0:64], pT_[:, ii, :],
                             start=(i == 0), stop=(i == nblk - 1), r=[pT_], w=[pd_], sig=(ii == ng - 1))
                S.v("dve", "reciprocal", rd_[:, :], pd_[0:64, 0:128], r=[pd_], w=[rd_])
                S.v("dve", "tensor_tensor", og[:, hh, :], po_[0:64, 0:128], rd_[:, :], ALU.mult, r=[po_, pd_, rd_], w=[og])
            S.dma("sp", oT_dv[:, 2 * pr:2 * pr + 2, j * 128:(j + 1) * 128], og[:, :, :], r=[og], w=[oT_d])
    SC.pop()
    SC.pop()
    if stop_after == "att":
        SC.push()
        tmpo = S.sb("tmpo", [64, 16 * 1024], BF16)
        d_ = dbg_out("oT", [64, 16 * 1024], BF16)
        S.dma("sp", tmpo[:, :], oT_d[:, :], r=[oT_d], w=[tmpo])
        S.dma("sp", d_[:, :], tmpo[:, :], r=[tmpo], w=[d_])
        SC.pop()
        SC.pop()
        return nc, dbg
    SC.push()
    xT = S.sb("xT", [128, 16, 1024], F32)
    h2T = S.sb("h2T", [128, 16, 1024], BF16)
    gbc = S.sb("gbc", [128, 8, 1024], BF16)
    S.dma("sp", xT[:, :, :], x1fm[:, :].rearrange("p (a b) -> p a b", a=16), r=[x1fm], w=[xT])
    SC.push()
    oT = S.sb("oT1", [64, 16, 1024], BF16)
    wbo = [S.sb(f"wbo{i}", [64, 16, 256], BF16) for i in range(2)]
    pa = [S.ps(f"pa{i}", [128, 512], F32) for i in range(3)]
    S.dma("sp", oT[:, :, :], oT_d[:, :].rearrange("p (h t) -> p h t", h=16), r=[oT_d], w=[oT])
    n = 0
    for fcg in range(8):
        w_ = wbo[fcg % 2]
        S.dma("pool", w_[:, :, :], b_w_out[:, fcg * 256:(fcg + 1) * 256].rearrange("(h d) n -> d h n", d=64),
              r=[b_w_out], w=[w_])
        for fc2 in range(2):
            fc = fcg * 2 + fc2
            for half in range(2):
                p_ = pa[n % 3]
                n += 1
                hs = slice(half * 512, (half + 1) * 512)
                for h in range(16):
                    S.mm(p_[:, :], w_[:, h, fc2 * 128:(fc2 + 1) * 128], oT[:, h, hs], start=(h == 0), stop=(h == 15),
                         r=[w_, oT], w=[p_])
                S.v("dve", "scalar_tensor_tensor", xT[:, fc, hs], p_[:, :], gt1[:, fc:fc + 1], xT[:, fc, hs],
                    ALU.mult, ALU.add, r=[p_, xT], w=[xT])
    SC.pop()
    if "xa" in debug:
        d_ = dbg_out("xa", [128, 16 * 1024])
        S.dma("sp", d_[:, :].rearrange("p (a b) -> p a b", a=16), xT[:, :, :], r=[xT], w=[d_])

    SC.push()
    rw = S.sb("rw", [128, 16, 8], F32)
    rbs = S.sb("rbs", [128, 8], F32)
    sels = S.sb("sels", [8, 8 * 128], F32)
    lg = S.sb("lg", [128, 8, 8], F32)
    dg = S.sb("dg", [128, 8, 8], F32)
    m8 = S.sb("m8", [128, 8, 8], F32)
    e2 = S.sb("e2", [128, 8, 4], F32)
    eq = S.sb("eq", [128, 8, 8], F32)
    dgT = S.sb("dgT", [8, 1024], F32)
    plog = [S.ps(f"plog{i}", [128, 512], F32) for i in range(4)]
    S.dma("sp", rw[:, :, :], router[:, :].rearrange("(c p) e -> p c e", p=128), r=[router], w=[rw])
    S.dma("sp", rbs[:, :], rb_bc[:, :], r=[rb_bc], w=[rbs])
    S.dma("sp", sels[:, :], selc[:, :], r=[selc], w=[sels])
    plog8 = [plog[i % 4] for i in range(8)]

    def norm_router():
        SC.push()
        rstd = S.sb("rstd_r", [128, 1024], F32)
        sq = [S.sb(f"sq{i}_r", [128, 512], BF16) for i in range(2)]
        tmp = [S.sb(f"tmpn{i}_r", [128, 512], F32) for i in range(2)]
        pss = [S.ps(f"pss{i}_r", [128, 512], F32) for i in range(2)]
        for half in range(2):
            cs_ = slice(half * 512, (half + 1) * 512)
            ps_ = pss[half]
            for c in range(16):
                q = sq[c % 2]
                S.act(q[:, :], xT[:, c, cs_], AF.Square, r=[xT], w=[q])
                S.mm(ps_[:, :], onesb[:, :], q[:, :], start=(c == 0), stop=(c == 15), r=[q], w=[ps_])
            S.act(rstd[:, cs_], ps_[:, :], AF.Sqrt, scale=1.0 / 2048, bias=epsc, r=[ps_], w=[rstd])
            S.v("dve", "reciprocal", rstd[:, cs_], rstd[:, cs_], r=[rstd], w=[rstd])
            for c in range(16):
                t = tmp[c % 2]
                S.v("dve", "scalar_tensor_tensor", t[:, :], xT[:, c, cs_], s2[:, c:c + 1], rstd[:, cs_],
                    ALU.mult, ALU.mult, r=[xT, rstd], w=[t])
                S.v("dve", "tensor_scalar", t[:, :], t[:, :], sh2[:, c:c + 1], None, ALU.add, r=[t], w=[t])
                S.act(h2T[:, c, cs_], t[:, :], AF.Copy, r=[t], w=[h2T])
                for b4 in range(4):
                    pl = plog[b4]
                    S.mm(pl[:, 0:8], t[:, b4 * 128:(b4 + 1) * 128], rw[:, c, :], start=(c == 0),
                         stop=(c == 15), r=[t, rw], w=[pl])
            for b4 in range(4):
                tb = half * 4 + b4
                S.v("dve", "tensor_tensor", lg[:, tb, :], plog[b4][:, 0:8], rbs[:, :], ALU.add,
                    r=[plog[b4], rbs], w=[lg])
        SC.pop()

    norm_router()
    if "lg" in debug:
        d_ = dbg_out("lg", [128, 64])
        S.dma("sp", d_[:, :].rearrange("p (a b) -> p a b", a=8), lg[:, :, :], r=[lg], w=[d_])
    for tb in range(8):
        S.v("dve", "max", m8[:, tb, :], lg[:, tb, :], r=[lg], w=[m8])
        S.v("dve", "tensor_tensor", e2[:, tb, 0:1], m8[:, tb, 1:2], m8[:, tb, 0:1], ALU.subtract, r=[m8], w=[e2])
        S.act(e2[:, tb, 0:1], e2[:, tb, 0:1], AF.Exp, r=[e2], w=[e2])
        S.v("dve", "tensor_scalar", e2[:, tb, 1:2], e2[:, tb, 0:1], 1.0, None, ALU.add, r=[e2], w=[e2])
        S.v("dve", "reciprocal", e2[:, tb, 1:2], e2[:, tb, 1:2], r=[e2], w=[e2])
        S.v("dve", "tensor_tensor", e2[:, tb, 2:3], e2[:, tb, 0:1], e2[:, tb, 1:2], ALU.mult, r=[e2], w=[e2])
        S.v("dve", "tensor_scalar", dg[:, tb, :], lg[:, tb, :], m8[:, tb, 0:1], e2[:, tb, 1:2], ALU.is_equal, ALU.mult,
            r=[lg, m8, e2], w=[dg])
        S.v("dve", "tensor_scalar", eq[:, tb, :], lg[:, tb, :], m8[:, tb, 1:2], e2[:, tb, 2:3], ALU.is_equal, ALU.mult,
            r=[lg, m8, e2], w=[eq])
        S.v("dve", "tensor_tensor", dg[:, tb, :], dg[:, tb, :], eq[:, tb, :], ALU.add, r=[dg, eq], w=[dg])
    for tb in range(8):
        pl = plog[tb % 4]
        S.tr(pl[0:8, 0:128], dg[:, tb, :], cf[:, 0:128], r=[dg], w=[pl])
        S.v("dve", "tensor_copy", dgT[:, tb * 128:(tb + 1) * 128], pl[0:8, 0:128], r=[pl], w=[dgT])
    for e in range(8):
        for half in range(2):
            pl = plog[(e * 2 + half) % 4]
            hs = slice(half * 512, (half + 1) * 512)
            S.mm(pl[:, :], sels[:, e * 128:(e + 1) * 128], dgT[:, hs], r=[sels, dgT], w=[pl])
            S.act(gbc[:, e, hs], pl[:, :], AF.Copy, r=[pl], w=[gbc])
    if "dg" in debug:
        d_ = dbg_out("dg", [128, 64])
        S.dma("sp", d_[:, :].rearrange("p (a b) -> p a b", a=8), dg[:, :, :], r=[dg], w=[d_])
    SC.pop()
    if stop_after == "router":
        SC.pop(); SC.pop()
        return nc, dbg

    SC.push()
    w1t = [S.sb(f"w1t{i}", [128, 16, 256], BF16) for i in range(3)]
    w3t = [S.sb(f"w3t{i}", [128, 16, 256], BF16) for i in range(3)]
    w2t = [S.sb(f"w2t{i}", [128, 2048], BF16) for i in range(5)]
    ug = [S.sb(f"ug{i}", [128, 4, 1024], BF16) for i in range(2)]
    sil = [S.sb(f"sil{i}", [128, 512], BF16) for i in range(2)]
    sil2 = [S.sb(f"silb{i}", [128, 512], BF16) for i in range(2)]
    p1 = [S.ps(f"p1{i}", [128, 512], F32) for i in range(2)]
    p3 = [S.ps(f"p3{i}", [128, 512], F32) for i in range(2)]
    py = [S.ps(f"py{i}", [128, 512], F32) for i in range(3)]
    n13 = 0
    ny = 0
    gi = 0
    nw = 0
    nw2 = 0
    for e in range(8):
        for g in range(14):
            u_ = ug[gi % 2]
            gi += 1
            a1s, a3s, a2s = [], [], []
            for t2 in range(2):
                a1, a3 = w1t[nw % 3], w3t[nw % 3]
                nw += 1
                wload(S, "pool", a1, a1[:, :, :], moe_w1, g * 512 + t2 * 256, 256, r0=e * 2048)
                wload(S, "pool", a3, a3[:, :, :], moe_w3, g * 512 + t2 * 256, 256, r0=e * 2048)
                a1s.append(a1)
                a3s.append(a3)
            for ci in range(4):
                a2 = w2t[nw2 % 5]
                nw2 += 1
                r0 = e * 7168 + g * 512 + ci * 128
                for hh in range(2):
                    S.dma("pool", a2[:, hh * 1024:(hh + 1) * 1024],
                          moe_w2[r0:r0 + 128, hh * 1024:(hh + 1) * 1024], r=[moe_w2], w=[a2])
                a2s.append(a2)
            for ci in range(4):
                a1, a3 = a1s[ci // 2], a3s[ci // 2]
                c = ci % 2
                for half in range(2):
                    hs = slice(half * 512, (half + 1) * 512)
                    q1, q3, sl, sl2 = p1[n13 % 2], p3[n13 % 2], sil[n13 % 2], sil2[n13 % 2]
                    n13 += 1
                    for kc in range(16):
                        S.mm(q1[:, :], a1[:, kc, c * 128:(c + 1) * 128], h2T[:, kc, hs], start=(kc == 0),
                             stop=(kc == 15), r=[a1, h2T], w=[q1])
                    for kc in range(16):
                        S.mm(q3[:, :], a3[:, kc, c * 128:(c + 1) * 128], h2T[:, kc, hs], start=(kc == 0),
                             stop=(kc == 15), r=[a3, h2T], w=[q3])
                    S.act(sl[:, :], q1[:, :], AF.Silu, r=[q1], w=[sl])
                    S.v("dve", "tensor_tensor", sl2[:, :], sl[:, :], gbc[:, e, hs], ALU.mult, r=[sl, gbc], w=[sl2])
                    S.v("dve", "tensor_tensor", u_[:, ci, hs], sl2[:, :], q3[:, :], ALU.mult, r=[sl2, q3], w=[u_])
            for fc in range(16):
                for half in range(2):
                    hs = slice(half * 512, (half + 1) * 512)
                    y_ = py[ny % 3]
                    ny += 1
                    for ci in range(4):
                        S.mm(y_[:, :], a2s[ci][:, fc * 128:(fc + 1) * 128], u_[:, ci, hs], start=(ci == 0),
                             stop=(ci == 3), r=[a2s[ci], u_], w=[y_])
                    S.v("dve", "scalar_tensor_tensor", xT[:, fc, hs], y_[:, :], gt2[:, fc:fc + 1], xT[:, fc, hs],
                        ALU.mult, ALU.add, r=[y_, xT], w=[xT])
    SC.pop()
    S.dma("sp", out_fm[:, :].rearrange("p (a b) -> p a b", a=16), xT[:, :, :], r=[xT], w=[out_fm])
    SC.pop()
    SC.pop()
    return nc, dbg


def prep_B(inp, resA):
    TL = l1_tiles()
    selc = np.zeros((8, 8 * 128), np.float32)
    for e in range(8):
        selc[e, e * 128:(e + 1) * 128] = 1.0
    maps = []
    for core in range(8):
        b, par = core // 2, core % 2
        own, prt = resA[core], resA[core ^ 1]
        kt = np.concatenate([np.asarray(own["kT_out"]).reshape(128, 24, 1024),
                             np.asarray(prt["kT_out"]).reshape(128, 24, 1024)], axis=2).reshape(128, 24 * 2048)
        vv = np.concatenate([np.asarray(own["v_out"]), np.asarray(prt["v_out"])], axis=0)
        gsm = np.zeros((128, 4), np.float32)
        gsm[:, 0] = np.tile(inp["b_g_qn"][0], 2)
        m = {
            "x1fm": np.asarray(own["x1_out"]),
            "KT_all": np.ascontiguousarray(kt),
            "V_all": np.ascontiguousarray(vv),
            "c_fm": fm16(inp["c"][b]),
            "consts": make_consts(par),
            "relrow": make_relrow(),
            "rel_bias": np.ascontiguousarray(inp["rel_bias"]),
            "gvb": np.concatenate([fm16(inp["g_attn"][1]), fm16(inp["g_ffn"][1])], axis=1),
            "gsmb": gsm,
            "maskl1": make_l1_masks(par),
            "selc": selc,
            "rb_bc": np.ascontiguousarray(np.broadcast_to(inp["moe_router_b"][0][None, :], (128, 8))),
            "w_mod1": inp["w_mod"][1], "b_mod1": inp["b_mod"][1][None, :],
            "b_w_q": inp["b_w_q"][0], "b_w_out": inp["b_w_out"][0],
            "router": inp["moe_router"][0],
            "moe_w1": inp["moe_w1"][0].reshape(8 * 2048, 7168),
            "moe_w3": inp["moe_w3"][0].reshape(8 * 2048, 7168),
            "moe_w2": inp["moe_w2"][0].reshape(8 * 7168, 2048),
        }
        maps.append(m)
    return maps


def assemble_out(resB):
    out = np.zeros((4, 2048, 2048), np.float32)
    for core in range(8):
        b, par = core // 2, core % 2
        o = np.asarray(resB[core]["out_fm"]).reshape(128, 16, 1024).transpose(2, 1, 0).reshape(8, 128, 2048)
        for j in range(8):
            blk = 2 * j + par
            out[b, blk * 128:(blk + 1) * 128] = o[j]
    return out


_CACHE = {}


def kernel(**inputs):
    inp = {k: np.asarray(v) for k, v in inputs.items()}
    if "F" not in _CACHE:
        _CACHE["F"] = build_fused()
    rb = run_bass_kernel_spmd(_CACHE["F"], prep_fused(inp), core_ids=list(range(8)))
    return assemble_out(rb.results)


def build_fused():
    nc = bass.Bass("TRN2", target_bir_lowering=False)
    S = Sched(nc)
    SC = Scope(S)
    ctx = {"nc": nc, "S": S, "SC": SC}
    build_A(ctx=ctx)
    S.prefix = "zx_"
    kT_out, v_out, KT_all, V_all = ctx["kT_out"], ctx["v_out"], ctx["KT_all"], ctx["V_all"]
    consts = S.ext_cache["consts"]
    Gk = S.dram("Gk", [8 * 128, 24 * 1024], BF16)
    Gv = S.dram("Gv", [8 * 1024, 3072], BF16)
    SC.push()
    groups = [list(range(8))]
    ccs = [S.es.enter_context(nc.semaphore(f"cc{i}")) for i in range(2)]

    def cc(i, src, dst):
        def f(e):
            ins = e.collective_compute("AllGather", ALU.bypass, replica_groups=groups,
                                       ins=[src.t.opt()], outs=[dst.t.opt()])
            ins.then_inc(ccs[i], CC_INC)
            e.wait_ge(ccs[i], CC_INC)
            return ins
        return f

    S.flush()
    S.op("pool", cc(0, kT_out, Gk), r=[kT_out], w=[], sig=False)
    S.op("pool", cc(1, v_out, Gv), r=[v_out], w=[], sig=False)
    KT_allv = KT_all[:, :].rearrange("p (a b) -> p a b", a=24)
    Gkv = Gk[:, :].rearrange("p (a b) -> p a b", a=24)

    def cpk(e):
        pid = e.partition_id()
        partner = pid + 1 - 2 * (pid % 2)
        return e.dma_start(out=KT_allv[:, :, 1024:2048], in_=Gkv[bass.ds(partner * 128, 128), :, :])

    def cpv(e):
        pid = e.partition_id()
        partner = pid + 1 - 2 * (pid % 2)
        return e.dma_start(out=V_all[1024:2048, :], in_=Gv[bass.ds(partner * 1024, 1024), :])

    S.op("pool", cpk, w=[KT_all], dma=True)
    S.op("pool", cpv, w=[V_all], dma=True)
    SC.pop()
    build_B(ctx=ctx)
    return nc


CC_INC = 1


def prep_fused(inp):
    A = prep_A(inp)
    TL = l1_tiles()
    selc = np.zeros((8, 8 * 128), np.float32)
    for e in range(8):
        selc[e, e * 128:(e + 1) * 128] = 1.0
    maps = []
    for core in range(8):
        b, par = core // 2, core % 2
        gsm = np.zeros((128, 4), np.float32)
        gsm[:, 0] = np.tile(inp["b_g_qn"][0], 2)
        m = dict(A[core])
        m.update({
            "gvb": np.concatenate([fm16(inp["g_attn"][1]), fm16(inp["g_ffn"][1])], axis=1),
            "gsmb": gsm,
            "maskl1": make_l1_masks(par),
            "selc": selc,
            "rb_bc": np.ascontiguousarray(np.broadcast_to(inp["moe_router_b"][0][None, :], (128, 8))),
            "w_mod1": inp["w_mod"][1], "b_mod1": inp["b_mod"][1][None, :],
            "b_w_q": inp["b_w_q"][0], "b_w_out": inp["b_w_out"][0],
            "router": inp["moe_router"][0],
            "moe_w1": inp["moe_w1"][0].reshape(8 * 2048, 7168),
            "moe_w3": inp["moe_w3"][0].reshape(8 * 2048, 7168),
            "moe_w2": inp["moe_w2"][0].reshape(8 * 7168, 2048),
        })
        maps.append(m)
    return maps
```
